# Optimizing a Trainium2 kernel written in Bass

```python
import jax, jax.numpy as jnp
from jax import lax
import numpy as np

D_MODEL = 2048
BATCH = 4
SEQ = 4096
DEPTH = 2

SCONV_DIM = D_MODEL // 2
CONV_K = 3
POOL_DIM = D_MODEL // 2
POOL_WINDOWS = (2, 4, 8, 16)
POOL_GROUPS = len(POOL_WINDOWS)
POOL_GROUP_DIM = POOL_DIM // POOL_GROUPS
ATT_HEADS = 8
HEAD_DIM = 128
ATT_DIM = ATT_HEADS * HEAD_DIM
MOBA_BLOCK = 256
MOBA_TOPK = 3
QUERY_CHUNK = 32
N_BRANCH = 3
IN_DIM = 3 * SCONV_DIM + POOL_DIM + 3 * ATT_DIM + N_BRANCH * D_MODEL
D_FF = 5632
RMS_EPS = 1e-6

kernel_name = 'hybrid_conv_pool_moba_macaron'


def rmsnorm(x, g):
    xf = x.astype(jnp.float32)
    y = xf * lax.rsqrt(jnp.mean(xf * xf, axis=-1, keepdims=True) + RMS_EPS)
    return (y * g.astype(jnp.float32)).astype(x.dtype)


def swiglu(h, w_in, w_out):
    a, b = jnp.split(h @ w_in, 2, axis=-1)
    return (jax.nn.silu(a) * b) @ w_out


def short_conv_mixer(a_b, a_c, a_x, conv_w):
    u = a_c * a_x
    s = u.shape[1]
    up = jnp.pad(u, ((0, 0), (CONV_K - 1, 0), (0, 0)))
    conv = sum(conv_w[j] * up[:, j:j + s] for j in range(CONV_K))
    return a_b * conv


def causal_pool_mixer(p, pool_w, pool_scale):
    b, s, _ = p.shape
    pf = p.astype(jnp.float32).reshape(b, s, POOL_GROUPS, POOL_GROUP_DIM)
    cs = jnp.cumsum(pf, axis=1)
    t1 = jnp.arange(1, s + 1, dtype=jnp.float32)
    outs = []
    for g, w in enumerate(POOL_WINDOWS):
        c = cs[:, :, g]
        prev = jnp.pad(c, ((0, 0), (w, 0), (0, 0)))[:, :s]
        mean = (c - prev) / jnp.minimum(t1, float(w))[None, :, None]
        outs.append(mean - pf[:, :, g])
    d = jnp.stack(outs, axis=2).astype(p.dtype)
    y = jnp.einsum('bsgc,gcd->bsgd', d, pool_w).reshape(b, s, POOL_DIM)
    return y * pool_scale


def moba_attention(q, k, v):
    b, s, _ = q.shape
    nb = -(-s // MOBA_BLOCK)
    sp = nb * MOBA_BLOCK
    n_sel = min(MOBA_TOPK, nb)

    def heads(z):
        return z.reshape(b, s, ATT_HEADS, HEAD_DIM).transpose(0, 2, 1, 3)

    qh = heads(q)
    pad = ((0, 0), (0, 0), (0, sp - s), (0, 0))
    kh = jnp.pad(heads(k), pad)
    vh = jnp.pad(heads(v), pad)
    kb = kh.reshape(b, ATT_HEADS, nb, MOBA_BLOCK, HEAD_DIM)
    vb = vh.reshape(b, ATT_HEADS, nb, MOBA_BLOCK, HEAD_DIM)
    kbar = jnp.mean(kb.astype(jnp.float32), axis=3)
    slopes = jnp.exp2(-(8.0 / ATT_HEADS) * jnp.arange(1, ATT_HEADS + 1, dtype=jnp.float32))
    scale = HEAD_DIM ** -0.5
    blk_ids = jnp.arange(nb)
    key_off = jnp.arange(MOBA_BLOCK)
    gather = jax.vmap(jax.vmap(lambda tab, idx: tab[idx]))

    def chunk(c):
        start = c * QUERY_CHUNK
        qc = lax.dynamic_slice_in_dim(qh, start, QUERY_CHUNK, axis=2)
        t = (start + jnp.arange(QUERY_CHUNK)).astype(jnp.float32)
        own = start // MOBA_BLOCK
        gate = jnp.einsum('bhqd,bhnd->bhqn', qc.astype(jnp.float32), kbar)
        gate = jnp.where(blk_ids < own, gate, -jnp.inf)
        _, sel = lax.top_k(gate, n_sel)
        valid = jnp.arange(n_sel) < own
        k_sel = gather(kb, sel)
        v_sel = gather(vb, sel)
        s_sel = jnp.einsum('bhqd,bhqjkd->bhqjk', qc, k_sel).astype(jnp.float32) * scale
        pos_sel = (sel[..., None] * MOBA_BLOCK + key_off).astype(jnp.float32)
        s_sel = s_sel - slopes[:, None, None, None] * (t[:, None, None] - pos_sel)
        s_sel = jnp.where(valid[:, None], s_sel, -jnp.inf)
        k_own = lax.dynamic_slice_in_dim(kh, own * MOBA_BLOCK, MOBA_BLOCK, axis=2)
        v_own = lax.dynamic_slice_in_dim(vh, own * MOBA_BLOCK, MOBA_BLOCK, axis=2)
        s_own = jnp.einsum('bhqd,bhkd->bhqk', qc, k_own).astype(jnp.float32) * scale
        dist = t[:, None] - (own * MOBA_BLOCK + key_off).astype(jnp.float32)[None, :]
        s_own = jnp.where(dist >= 0, s_own - slopes[:, None, None] * dist, -jnp.inf)
        scores = jnp.concatenate(
            [s_sel.reshape(b, ATT_HEADS, QUERY_CHUNK, n_sel * MOBA_BLOCK), s_own], axis=-1)
        pr = jax.nn.softmax(scores, axis=-1).astype(v.dtype)
        p_sel = pr[..., :n_sel * MOBA_BLOCK].reshape(b, ATT_HEADS, QUERY_CHUNK, n_sel, MOBA_BLOCK)
        p_own = pr[..., n_sel * MOBA_BLOCK:]
        return (jnp.einsum('bhqjk,bhqjkd->bhqd', p_sel, v_sel)
                + jnp.einsum('bhqk,bhkd->bhqd', p_own, v_own))

    out = lax.map(chunk, jnp.arange(s // QUERY_CHUNK))
    return out.transpose(1, 0, 3, 2, 4).reshape(b, s, ATT_DIM)


def hybrid_mixer(h, w_in, conv_w, conv_w_out, pool_w, pool_scale, pool_w_out, attn_w_out, w_out):
    b, s, _ = h.shape
    proj = h @ w_in
    splits = np.cumsum([SCONV_DIM, SCONV_DIM, SCONV_DIM, POOL_DIM, ATT_DIM, ATT_DIM, ATT_DIM]).tolist()
    a_b, a_c, a_x, p, q, k, v, g = jnp.split(proj, splits, axis=-1)
    y_a = short_conv_mixer(a_b, a_c, a_x, conv_w) @ conv_w_out
    y_b = causal_pool_mixer(p, pool_w, pool_scale) @ pool_w_out
    y_c = moba_attention(q, k, v) @ attn_w_out
    gates = jax.nn.sigmoid(g.astype(jnp.float32)).astype(h.dtype).reshape(b, s, N_BRANCH, D_MODEL)
    merged = gates[:, :, 0] * y_a + gates[:, :, 1] * y_b + gates[:, :, 2] * y_c
    return merged @ w_out


def setup_inputs(seed: int = 0) -> dict:
    key = jax.random.key(seed)
    ks = jax.random.split(key, 20)
    f32 = jnp.float32

    def w(k, shape, fan_in):
        return jax.random.normal(k, shape, f32) * (fan_in ** -0.5)

    def gain(k, shape, noise):
        return 1.0 + noise * jax.random.normal(k, shape, f32)

    L = DEPTH
    return {
        'x': jax.random.normal(ks[0], (BATCH, SEQ, D_MODEL), f32),
        'ffn1_norm': gain(ks[1], (L, D_MODEL), 0.05),
        'ffn1_w_in': w(ks[2], (L, D_MODEL, 2 * D_FF), D_MODEL),
        'ffn1_w_out': w(ks[3], (L, D_FF, D_MODEL), D_FF),
        'mix_norm': gain(ks[4], (L, D_MODEL), 0.05),
        'mix_w_in': w(ks[5], (L, D_MODEL, IN_DIM), D_MODEL),
        'conv_w': w(ks[6], (L, CONV_K, SCONV_DIM), CONV_K),
        'conv_w_out': w(ks[7], (L, SCONV_DIM, D_MODEL), SCONV_DIM),
        'pool_w': w(ks[8], (L, POOL_GROUPS, POOL_GROUP_DIM, POOL_GROUP_DIM), POOL_GROUP_DIM),
        'pool_scale': gain(ks[9], (L, POOL_DIM), 0.1),
        'pool_w_out': w(ks[10], (L, POOL_DIM, D_MODEL), POOL_DIM),
        'attn_w_out': w(ks[11], (L, ATT_DIM, D_MODEL), ATT_DIM),
        'mix_w_out': w(ks[12], (L, D_MODEL, D_MODEL), D_MODEL),
        'ffn2_norm': gain(ks[13], (L, D_MODEL), 0.05),
        'ffn2_w_in': w(ks[14], (L, D_MODEL, 2 * D_FF), D_MODEL),
        'ffn2_w_out': w(ks[15], (L, D_FF, D_MODEL), D_FF),
        'final_norm': gain(ks[16], (D_MODEL,), 0.05),
    }


def reference(x, ffn1_norm, ffn1_w_in, ffn1_w_out, mix_norm, mix_w_in, conv_w, conv_w_out,
              pool_w, pool_scale, pool_w_out, attn_w_out, mix_w_out, ffn2_norm, ffn2_w_in,
              ffn2_w_out, final_norm):
    for l in range(DEPTH):
        x = x + 0.5 * swiglu(rmsnorm(x, ffn1_norm[l]), ffn1_w_in[l], ffn1_w_out[l])
        x = x + hybrid_mixer(rmsnorm(x, mix_norm[l]), mix_w_in[l], conv_w[l], conv_w_out[l],
                             pool_w[l], pool_scale[l], pool_w_out[l], attn_w_out[l], mix_w_out[l])
        x = x + 0.5 * swiglu(rmsnorm(x, ffn2_norm[l]), ffn2_w_in[l], ffn2_w_out[l])
    return rmsnorm(x, final_norm)
```

```python
from contextlib import ExitStack, contextmanager
import numpy as np
import concourse.bass as bass
import concourse.mybir as mybir
from concourse.bass_utils import run_bass_kernel_spmd

F32 = mybir.dt.float32
BF16 = mybir.dt.bfloat16
AF = mybir.ActivationFunctionType
ALU = mybir.AluOpType
AX = mybir.AxisListType

D = 2048
DFF = 5632
NL = 2
INDIM = 13312
NH = 8
EPS = 1e-6
NEG = -1.0e30
PEN = -30000.0
DBG = {}


class Buf:
    __slots__ = ("name", "w", "r", "dsem")

    def __init__(self, name):
        self.name = name
        self.w = None
        self.r = {}
        self.dsem = None


class SemSlot:
    __slots__ = ("key", "cnt")

    def __init__(self, key):
        self.key = key
        self.cnt = 0


class Ctx:
    NDSEM = 56

    def __init__(self, nc):
        self.nc = nc
        self.es = ExitStack()
        self.pes = None
        self.eng = {"pe": nc.tensor, "act": nc.scalar, "dve": nc.vector,
                    "pool": nc.gpsimd, "sp": nc.sync}
        self.semh = {}
        self.cnt = {}
        for k in self.eng:
            self.semh[k] = self.es.enter_context(nc.semaphore("s_" + k))
            self.cnt[k] = 0
        self.known = {k: {} for k in self.eng}
        self.slots = []
        for i in range(self.NDSEM):
            key = "d%d" % i
            self.semh[key] = self.es.enter_context(nc.semaphore(key))
            self.slots.append(SemSlot(key))
        self.free_slots = list(self.slots)
        self.phase_slots = []
        self.nwaits = 0
        self.ninstr = 0
        self.uid = 0

    def sbuf(self, name, shape, dt, persistent=False):
        self.uid += 1
        st = self.es if (persistent or self.pes is None) else self.pes
        return st.enter_context(self.nc.sbuf_tensor("%s_%d" % (name, self.uid), list(shape), dt))

    def psum(self, name, shape, dt):
        self.uid += 1
        return self.pes.enter_context(self.nc.psum_tensor("%s_%d" % (name, self.uid), list(shape), dt))

    def _wait(self, E, deps):
        eng = self.eng[E]
        kn = self.known[E]
        best = {}
        for d in deps:
            if d is None:
                continue
            k, v = d
            if E == "pe" and k == "pe":
                continue
            if best.get(k, 0) < v:
                best[k] = v
        for k, v in best.items():
            if kn.get(k, 0) < v:
                eng.wait_ge(self.semh[k], v)
                kn[k] = v
                self.nwaits += 1

    @staticmethod
    def _deps(reads, writes):
        deps = []
        for b in reads:
            deps.append(b.w)
        for b in writes:
            deps.append(b.w)
            deps.extend(b.r.items())
        return deps

    def op(self, E, fn, reads=(), writes=()):
        self._wait(E, self._deps(reads, writes))
        ins = fn()
        self.cnt[E] += 1
        cn = self.cnt[E]
        ins.then_inc(self.semh[E], 1)
        self.ninstr += 1
        for b in reads:
            if b.r.get(E, 0) < cn:
                b.r[E] = cn
        for b in writes:
            b.w = (E, cn)
            b.r = {}
        return ins

    def dma(self, out_ap, in_ap, reads=(), writes=(), sem_buf=None, Q="sp", **kw):
        self._wait(Q, self._deps(reads, writes))
        sb = sem_buf
        if sb.dsem is None:
            sb.dsem = self.free_slots.pop()
            self.phase_slots.append(sb.dsem)
        ins = self.eng[Q].dma_start(out=out_ap, in_=in_ap, **kw)
        sb.dsem.cnt += 16
        ins.then_inc(self.semh[sb.dsem.key], 16)
        self.ninstr += 1
        ev = (sb.dsem.key, sb.dsem.cnt)
        for b in reads:
            if b.r.get(ev[0], 0) < ev[1]:
                b.r[ev[0]] = ev[1]
        for b in writes:
            b.w = ev
            b.r = {}
        return ins

    def barrier(self):
        allev = [(k, v) for k, v in self.cnt.items() if v > 0]
        allev += [(s.key, s.cnt) for s in self.slots if s.cnt > 0]
        for E in self.eng:
            self._wait(E, allev)

    @contextmanager
    def phase(self):
        self.pes = ExitStack()
        try:
            yield
            self.barrier()
        finally:
            self.pes.close()
            self.pes = None
            self.free_slots.extend(self.phase_slots)
            self.phase_slots = []


class Ring:
    def __init__(self, c, name, shape, dt, n, psum=False):
        self.t = []
        self.b = []
        for i in range(n):
            nm = "%s%d" % (name, i)
            self.t.append(c.psum(nm, shape, dt) if psum else c.sbuf(nm, shape, dt))
            self.b.append(Buf(nm))
        self.i = 0
        self.n = n

    def next(self):
        i = self.i % self.n
        self.i += 1
        return self.t[i], self.b[i]


def cast_weight(c, src, K, runs, dst, eng_rot):
    nc = c.nc
    KC = K // 128
    with c.phase():
        maxc = 3328
        fin = Ring(c, "cw_in", [128, maxc], F32, 3)
        fout = Ring(c, "cw_out", [128, maxc], BF16, 3)
        for kc in range(KC):
            for (col0, nseg, w, s0, c0) in runs:
                per = max(1, maxc // w)
                sg = 0
                while sg < nseg:
                    n = min(per, nseg - sg)
                    ncols = n * w
                    a = col0 + sg * w
                    ti, bi = fin.next()
                    to, bo = fout.next()
                    c.dma(ti[:, 0:ncols], src[kc * 128:(kc + 1) * 128, a:a + ncols], writes=[bi], sem_buf=bi)
                    E = eng_rot[0]
                    eng_rot.append(eng_rot.pop(0))
                    if E == "act":
                        c.op("act", lambda: nc.scalar.copy(to[:, 0:ncols], ti[:, 0:ncols]), reads=[bi], writes=[bo])
                    elif E == "dve":
                        c.op("dve", lambda: nc.vector.tensor_copy(to[:, 0:ncols], ti[:, 0:ncols]), reads=[bi], writes=[bo])
                    else:
                        c.op("pool", lambda: nc.gpsimd.tensor_copy(to[:, 0:ncols], ti[:, 0:ncols]), reads=[bi], writes=[bo])
                    dv = dst[s0 + sg:s0 + sg + n, :, kc, c0:c0 + w].rearrange("s p c -> p s c")
                    c.dma(dv, to[:, 0:ncols].rearrange("p (s c) -> p s c", c=w), reads=[bo], sem_buf=bo)
                    sg += n


def norm_phase(c, K, src, dst, dst_dt, gain_ap, T):
    nc = c.nc
    TT = 256
    srcv = src.rearrange("(k p) t -> p k t", p=128)
    dstv = dst.rearrange("(k p) t -> p k t", p=128)
    with c.phase():
        xr = Ring(c, "nx", [128, 16, TT], F32, 2)
        sr = Ring(c, "nsq", [128, 16, TT], F32, 2)
        hr = Ring(c, "nh", [128, 16, TT], dst_dt, 2)
        rr = Ring(c, "nr", [128, TT], F32, 2)
        pr = Ring(c, "nps", [128, 512], F32, 2, psum=True)
        for tt in range(T // TT):
            x, bx = xr.next()
            sq, bs = sr.next()
            h, bh = hr.next()
            r, br = rr.next()
            ps, bp = pr.next()
            c.dma(x[:], srcv[:, :, tt * TT:(tt + 1) * TT], writes=[bx], sem_buf=bx)
            c.op("act", lambda: nc.scalar.activation(sq[:], x[:], AF.Square), reads=[bx], writes=[bs])
            for k in range(16):
                c.op("pe", lambda: nc.tensor.matmul(ps[:, 0:TT], K.ones_f[:], sq[:, k, :], start=(k == 0), stop=(k == 15)),
                     reads=[bs], writes=[bp])
            c.op("dve", lambda: nc.vector.tensor_scalar(r[:], ps[:, 0:TT], 1.0 / D, EPS, ALU.mult, ALU.add),
                 reads=[bp], writes=[br])
            c.op("act", lambda: nc.scalar.activation(r[:], r[:], AF.Sqrt), reads=[br], writes=[br])
            c.op("dve", lambda: nc.vector.reciprocal(r[:], r[:]), reads=[br], writes=[br])
            for k in range(16):
                c.op("dve", lambda: nc.vector.scalar_tensor_tensor(h[:, k, :], x[:, k, :], gain_ap[:, k:k + 1], r[:], ALU.mult, ALU.mult),
                     reads=[bx, br], writes=[bh])
            c.dma(dstv[:, :, tt * TT:(tt + 1) * TT], h[:], reads=[bh], sem_buf=bh)


def linear_phase(c, srcT, Kdim, wsl, S, C, TS, T, epi, nact=2, tokmajor=()):
    nc = c.nc
    KC = Kdim // 128
    G = C // 128
    nsup = T // TS
    nsub = TS // 512
    srcv = srcT.rearrange("(k p) t -> p k t", p=128)
    with c.phase():
        act = Ring(c, "act", [128, KC, TS], BF16, nact)
        wt = Ring(c, "wt", [128, KC, C], BF16, 2)
        banks = Ring(c, "bk", [128, 512], F32, 8, psum=True)
        epi.setup(c)
        seq = [(su, s) for su in range(nsup) for s in range(S)]
        acts = {}

        def load_act(su):
            a, ba = act.next()
            for k0 in range(0, KC, 8):
                k1 = min(KC, k0 + 8)
                c.dma(a[:, k0:k1, :], srcv[:, k0:k1, su * TS:(su + 1) * TS], writes=[ba], sem_buf=ba)
            acts[su] = (a, ba)

        wts = {}

        def load_w(i):
            w, bw = wt.next()
            c.dma(w[:], wsl[seq[i][1]], writes=[bw], sem_buf=bw)
            wts[i] = (w, bw)

        load_act(0)
        load_w(0)
        for i, (su, s) in enumerate(seq):
            if s == 0 and su + 1 < nsup and nact > 1:
                load_act(su + 1)
            if su not in acts:
                load_act(su)
            if i + 1 < len(seq):
                load_w(i + 1)
            w, bw = wts.pop(i)
            a, ba = acts[su]
            for sb in range(nsub):
                t0 = su * TS + sb * 512
                bl = [banks.next() for _ in range(4 if s in tokmajor else G)]
                if s in tokmajor:
                    for g in range(4):
                        for k in range(KC):
                            c.op("pe", lambda: nc.tensor.matmul(bl[g][0][:, 0:C], a[:, k, sb * 512 + g * 128:sb * 512 + (g + 1) * 128],
                                                                w[:, k, :], start=(k == 0), stop=(k == KC - 1)),
                                 reads=[ba, bw], writes=[bl[g][1]])
                else:
                    for g in range(G):
                        for k in range(KC):
                            c.op("pe", lambda: nc.tensor.matmul(bl[g][0][:], w[:, k, g * 128:(g + 1) * 128],
                                                                a[:, k, sb * 512:(sb + 1) * 512], start=(k == 0), stop=(k == KC - 1)),
                                 reads=[ba, bw], writes=[bl[g][1]])
                epi(c, s, t0, [b[0] for b in bl], [b[1] for b in bl])
        epi.finish(c)


class EpiFfnIn:
    def __init__(self, gT):
        self.gT = gT

    def setup(self, c):
        self.sa = Ring(c, "e_sa", [128, 512], F32, 4)
        self.go = Ring(c, "e_go", [128, 2, 512], BF16, 3)

    def __call__(self, c, s, t0, bk, bb):
        nc = c.nc
        go, bgo = self.go.next()
        for j in range(2):
            sa, bsa = self.sa.next()
            c.op("act", lambda: nc.scalar.activation(sa[:], bk[j][:], AF.Silu), reads=[bb[j]], writes=[bsa])
            c.op("dve", lambda: nc.vector.tensor_tensor(go[:, j, :], sa[:], bk[2 + j][:], ALU.mult),
                 reads=[bsa, bb[2 + j]], writes=[bgo])
        dv = self.gT[s * 256:(s + 1) * 256, t0:t0 + 512].rearrange("(j p) t -> p j t", p=128)
        c.dma(dv, go[:], reads=[bgo], sem_buf=bgo)

    def finish(self, c):
        pass


class EpiResid:
    def __init__(self, xsrc, xdst, coef):
        self.xsrc, self.xdst, self.coef = xsrc, xdst, coef

    def setup(self, c):
        self.xr = Ring(c, "e_x", [128, 2, 512], F32, 4)

    def __call__(self, c, s, t0, bk, bb):
        nc = c.nc
        x, bx = self.xr.next()
        sv = self.xsrc[s * 256:(s + 1) * 256, t0:t0 + 512].rearrange("(j p) t -> p j t", p=128)
        dv = self.xdst[s * 256:(s + 1) * 256, t0:t0 + 512].rearrange("(j p) t -> p j t", p=128)
        c.dma(x[:], sv, writes=[bx], sem_buf=bx)
        for j in range(2):
            c.op("dve", lambda: nc.vector.scalar_tensor_tensor(x[:, j, :], bk[j][:], float(self.coef), x[:, j, :], ALU.mult, ALU.add),
                 reads=[bb[j], bx], writes=[bx])
        c.dma(dv, x[:], reads=[bx], sem_buf=bx)

    def finish(self, c):
        pass


class EpiMixIn:
    def __init__(self, pa, qk, v, kbar, sg, T):
        self.pa, self.qk, self.v, self.kbar, self.sg, self.T = pa, qk, v, kbar, sg, T

    def setup(self, c):
        self.f = Ring(c, "e_f", [128, 4, 512], F32, 2)
        self.h = Ring(c, "e_h", [128, 4, 512], BF16, 3)
        self.kf = Ring(c, "e_kf", [128, 512], F32, 2)
        self.kb = c.sbuf("e_kb", [128, 8, self.T // 256], F32)
        self.bkb = Buf("e_kb")
        self.kbo = c.sbuf("e_kbo", [128, 8, 16], BF16)
        self.bkbo = Buf("e_kbo")
        self.n = 0

    def __call__(self, c, s, t0, bk, bb):
        nc = c.nc
        self.n += 1
        if s < 8:
            f, bf = self.f.next()
            for j in range(4):
                if (self.n + j) % 2 == 0:
                    c.op("act", lambda: nc.scalar.copy(f[:, j, :], bk[j][:]), reads=[bb[j]], writes=[bf])
                else:
                    c.op("dve", lambda: nc.vector.tensor_copy(f[:, j, :], bk[j][:]), reads=[bb[j]], writes=[bf])
            dv = self.pa[s * 512:(s + 1) * 512, t0:t0 + 512].rearrange("(j p) t -> p j t", p=128)
            c.dma(dv, f[:], reads=[bf], sem_buf=bf)
        elif s < 12:
            h, bh = self.h.next()
            for j in range(4):
                if s < 10:
                    if (self.n + j) % 2 == 0:
                        c.op("act", lambda: nc.scalar.copy(h[:, j, :], bk[j][:]), reads=[bb[j]], writes=[bh])
                    else:
                        c.op("dve", lambda: nc.vector.tensor_copy(h[:, j, :], bk[j][:]), reads=[bb[j]], writes=[bh])
                else:
                    hd = (s - 10) * 4 + j
                    b0 = t0 // 256
                    kf, bkf = self.kf.next()
                    c.op("act", lambda: nc.scalar.copy(kf[:], bk[j][:]), reads=[bb[j]], writes=[bkf])
                    c.op("pool", lambda: nc.gpsimd.tensor_copy(h[:, j, :], kf[:]), reads=[bkf], writes=[bh])
                    for hb in range(2):
                        c.op("dve", lambda: nc.vector.reduce_sum(self.kb[:, hd, b0 + hb:b0 + hb + 1], kf[:, hb * 256:(hb + 1) * 256], AX.X),
                             reads=[bkf], writes=[self.bkb])
            r0 = (s - 8) * 512
            dv = self.qk[r0:r0 + 512, t0:t0 + 512].rearrange("(j p) t -> p j t", p=128)
            c.dma(dv, h[:], reads=[bh], sem_buf=bh)
        elif s < 14:
            h, bh = self.h.next()
            for g in range(4):
                if (self.n + g) % 2 == 0:
                    c.op("act", lambda: nc.scalar.copy(h[:, g, :], bk[g][:]), reads=[bb[g]], writes=[bh])
                else:
                    c.op("dve", lambda: nc.vector.tensor_copy(h[:, g, :], bk[g][:]), reads=[bb[g]], writes=[bh])
            c0 = (s - 12) * 512
            dv = self.v[t0:t0 + 512, c0:c0 + 512].rearrange("(g p) f -> p g f", p=128)
            c.dma(dv, h[:], reads=[bh], sem_buf=bh)
        else:
            h, bh = self.h.next()
            for j in range(4):
                c.op("act", lambda: nc.scalar.activation(h[:, j, :], bk[j][:], AF.Sigmoid), reads=[bb[j]], writes=[bh])
            r0 = (s - 14) * 512
            dv = self.sg[r0:r0 + 512, t0:t0 + 512].rearrange("(j p) t -> p j t", p=128)
            c.dma(dv, h[:], reads=[bh], sem_buf=bh)

    def finish(self, c):
        nc = c.nc
        NB = self.T // 256
        c.op("dve", lambda: nc.vector.memset(self.kbo[:], 0.0), writes=[self.bkbo])
        c.op("dve", lambda: nc.vector.tensor_scalar(self.kbo[:, :, 0:NB], self.kb[:], 1.0 / 256, None, ALU.mult),
             reads=[self.bkb], writes=[self.bkbo])
        c.dma(self.kbar.rearrange("p (h n) -> p h n", n=16), self.kbo[:], reads=[self.bkbo], sem_buf=self.bkbo)


def conv_phase(c, K, l, pa, ya, T):
    nc = c.nc
    TT = 1024
    cxv = pa[1024:3072, :].rearrange("(two c) t -> c two t", two=2)
    with c.phase():
        cxr = Ring(c, "cv_cx", [128, 2, TT + 2], F32, 2)
        btr = Ring(c, "cv_b", [128, TT], F32, 2)
        ur = Ring(c, "cv_u", [128, TT + 2], F32, 2)
        ar = Ring(c, "cv_a", [128, TT], F32, 2)
        yr = Ring(c, "cv_y", [128, TT], BF16, 2)
        for ch in range(8):
            wb = (l * 8 + ch) * 3
            for tt in range(T // TT):
                t0 = tt * TT
                cx, bcx = cxr.next()
                bt, bbt = btr.next()
                u, bu = ur.next()
                a, ba = ar.next()
                y, by = yr.next()
                if t0 == 0:
                    c.op("pool", lambda: nc.gpsimd.memset(cx[:, :, 0:2], 0.0), writes=[bcx])
                    c.dma(cx[:, :, 2:], cxv[ch * 128:(ch + 1) * 128, :, 0:TT], writes=[bcx], sem_buf=bcx)
                else:
                    c.dma(cx[:], cxv[ch * 128:(ch + 1) * 128, :, t0 - 2:t0 + TT], writes=[bcx], sem_buf=bcx)
                c.dma(bt[:], pa[ch * 128:(ch + 1) * 128, t0:t0 + TT], writes=[bbt], sem_buf=bbt)
                c.op("pool", lambda: nc.gpsimd.tensor_tensor(u[:], cx[:, 0, :], cx[:, 1, :], ALU.mult), reads=[bcx], writes=[bu])
                c.op("dve", lambda: nc.vector.tensor_scalar(a[:], u[:, 2:TT + 2], K.convw[:, wb + 2:wb + 3], None, ALU.mult),
                     reads=[bu], writes=[ba])
                c.op("dve", lambda: nc.vector.scalar_tensor_tensor(a[:], u[:, 1:TT + 1], K.convw[:, wb + 1:wb + 2], a[:], ALU.mult, ALU.add),
                     reads=[bu, ba], writes=[ba])
                c.op("dve", lambda: nc.vector.scalar_tensor_tensor(a[:], u[:, 0:TT], K.convw[:, wb:wb + 1], a[:], ALU.mult, ALU.add),
                     reads=[bu, ba], writes=[ba])
                c.op("pool", lambda: nc.gpsimd.tensor_tensor(y[:], bt[:], a[:], ALU.mult), reads=[bbt, ba], writes=[by])
                c.dma(ya[ch * 128:(ch + 1) * 128, t0:t0 + TT], y[:], reads=[by], sem_buf=by)


def pool_phase(c, K, l, pa, pool_w_l, yb, T):
    nc = c.nc
    TT = 512
    H = 16
    with c.phase():
        pwf = c.sbuf("pl_wf", [128, 4, 2, 256], F32)
        pwb = c.sbuf("pl_wb", [128, 4, 2, 256], BF16)
        bpwf, bpwb = Buf("pl_wf"), Buf("pl_wb")
        c.dma(pwf[:], pool_w_l.rearrange("g (k p) d -> p g k d", p=128), writes=[bpwf], sem_buf=bpwf)
        c.op("dve", lambda: nc.vector.tensor_copy(pwb[:], pwf[:]), reads=[bpwf], writes=[bpwb])
        pr = Ring(c, "pl_p", [128, 2, TT + H], F32, 2)
        s1r = Ring(c, "pl_s1", [128, 2, TT + H], F32, 2)
        s2r = Ring(c, "pl_s2", [128, 2, TT + H], F32, 2)
        dr = Ring(c, "pl_d", [128, 2, TT], BF16, 2)
        yr = Ring(c, "pl_y", [128, 2, TT], BF16, 2)
        banks = Ring(c, "pl_bk", [128, 512], F32, 4, psum=True)
        for g in range(4):
            w = 2 << g
            pv = pa[3072 + g * 256:3072 + (g + 1) * 256, :].rearrange("(ci p) t -> p ci t", p=128)
            for tt in range(T // TT):
                t0 = tt * TT
                p, bp = pr.next()
                s1, bs1 = s1r.next()
                s2, bs2 = s2r.next()
                d, bd = dr.next()
                y, by = yr.next()
                if t0 == 0:
                    c.op("pool", lambda: nc.gpsimd.memset(p[:, :, 0:H], 0.0), writes=[bp])
                    c.dma(p[:, :, H:], pv[:, :, 0:TT], writes=[bp], sem_buf=bp)
                else:
                    c.dma(p[:], pv[:, :, t0 - H:t0 + TT], writes=[bp], sem_buf=bp)
                cur, bcur = p, bp
                sh = 1
                tog = 0
                while sh < w:
                    nxt, bnxt = (s1, bs1) if tog == 0 else (s2, bs2)
                    tog ^= 1
                    E = "dve" if tog else "pool"
                    eng = nc.vector if E == "dve" else nc.gpsimd
                    c.op(E, lambda: eng.tensor_tensor(nxt[:, :, sh:], cur[:, :, sh:], cur[:, :, 0:TT + H - sh], ALU.add),
                         reads=[bcur], writes=[bnxt])
                    cur, bcur = nxt, bnxt
                    sh *= 2
                if t0 == 0:
                    for ci in range(2):
                        c.op("dve", lambda: nc.vector.tensor_tensor(cur[:, ci, H:], cur[:, ci, H:], K.invd[:, g, :], ALU.mult),
                             reads=[bcur], writes=[bcur])
                    c.op("dve", lambda: nc.vector.tensor_tensor(d[:], cur[:, :, H:], p[:, :, H:], ALU.subtract),
                         reads=[bcur, bp], writes=[bd])
                else:
                    c.op("dve", lambda: nc.vector.scalar_tensor_tensor(d[:], cur[:, :, H:], 1.0 / w, p[:, :, H:], ALU.mult, ALU.subtract),
                         reads=[bcur, bp], writes=[bd])
                for j in range(2):
                    bk, bbk = banks.next()
                    for ci in range(2):
                        c.op("pe", lambda: nc.tensor.matmul(bk[:], pwb[:, g, ci, j * 128:(j + 1) * 128], d[:, ci, :],
                                                            start=(ci == 0), stop=(ci == 1)),
                             reads=[bpwb, bd], writes=[bbk])
                    sc = l * 8 + g * 2 + j
                    c.op("act", lambda: nc.scalar.activation(y[:, j, :], bk[:], AF.Copy, scale=K.pscale[:, sc:sc + 1]),
                         reads=[bbk], writes=[by])
                dv = yb[g * 256:(g + 1) * 256, t0:t0 + TT].rearrange("(j p) t -> p j t", p=128)
                c.dma(dv, y[:], reads=[by], sem_buf=by)


def attn_phase(c, K, qk, v, kbar, attnT, T):
    nc = c.nc
    NQ = T // 128
    scale = 128.0 ** -0.5
    vv = v.rearrange("(c p) f -> p c f", p=128)
    with c.phase():
        qr = Ring(c, "at_q", [128, T], BF16, 2)
        kr = Ring(c, "at_k", [128, T], BF16, 2)
        vr = Ring(c, "at_v", [128, NQ, 128], BF16, 2)
        kbr = Ring(c, "at_kb", [128, 16], BF16, 2)
        gm = c.sbuf("at_gm", [128, NQ, 16], F32)
        bgm = Buf("at_gm")
        top = c.sbuf("at_top", [128, NQ, 8], F32)
        btop = Buf("at_top")
        thr = c.sbuf("at_thr", [128, NQ], F32)
        bthr = Buf("at_thr")
        pen = c.sbuf("at_pen", [128, NQ, 16], BF16)
        bpen = Buf("at_pen")
        penT = c.sbuf("at_penT", [128, T], BF16)
        bpenT = Buf("at_penT")
        c.op("pool", lambda: nc.gpsimd.memset(penT[:], 0.0), writes=[bpenT])
        ptr = Ring(c, "at_pt", [128, 4, 128], BF16, 3)
        rir = Ring(c, "at_ri", [128, 128], F32, 2)
        osr = Ring(c, "at_os", [128, T], BF16, 2)
        gbank = c.psum("at_gb", [128, 512], F32)
        bgbank = Buf("at_gb")
        tbank = c.psum("at_tb", [16, 1024], BF16)
        btbank = Buf("at_tb")
        sbk = Ring(c, "at_s", [128, 4, 128], F32, 2, psum=True)
        obk = Ring(c, "at_o", [128, 512], F32, 2, psum=True)
        rbk = Ring(c, "at_r", [128, 512], F32, 2, psum=True)

        heads = {}

        def load_head(h):
            q, bq = qr.next()
            k, bk_ = kr.next()
            vt, bv = vr.next()
            kb, bkb = kbr.next()
            c.dma(q[:], qk[h * 128:(h + 1) * 128, :], writes=[bq], sem_buf=bq)
            c.dma(k[:], qk[1024 + h * 128:1024 + (h + 1) * 128, :], writes=[bk_], sem_buf=bk_)
            for c0 in range(0, NQ, 8):
                c.dma(vt[:, c0:c0 + 8, :], vv[:, c0:c0 + 8, h * 128:(h + 1) * 128], writes=[bv], sem_buf=bv)
            c.dma(kb[:], kbar[:, h * 16:(h + 1) * 16], writes=[bkb], sem_buf=bkb)
            heads[h] = (q, bq, k, bk_, vt, bv, kb, bkb)

        load_head(0)
        for h in range(NH):
            if h + 1 < NH:
                load_head(h + 1)
            q, bq, k, bk_, vt, bv, kb, bkb = heads.pop(h)
            for i in range(NQ):
                c.op("pe", lambda: nc.tensor.matmul(gbank[:, i * 16:(i + 1) * 16], q[:, i * 128:(i + 1) * 128], kb[:],
                                                    start=True, stop=True), reads=[bq, bkb], writes=[bgbank])
            c.op("dve", lambda: nc.vector.tensor_tensor(gm[:].rearrange("p a b -> p (a b)"), gbank[:, 0:NQ * 16], K.negmask[:], ALU.add),
                 reads=[bgbank], writes=[bgm])
            for i in range(NQ):
                c.op("dve", lambda: nc.vector.max(out=top[:, i, :], in_=gm[:, i, :]), reads=[bgm], writes=[btop])
            c.op("dve", lambda: nc.vector.tensor_scalar(thr[:], top[:, :, 2], -1.0e29, None, ALU.max), reads=[btop], writes=[bthr])
            for i in range(NQ):
                c.op("dve", lambda: nc.vector.tensor_scalar(pen[:, i, :], gm[:, i, :], thr[:, i:i + 1], PEN, ALU.is_lt, ALU.mult),
                     reads=[bgm, bthr], writes=[bpen])
            for i0 in range(0, NQ, 8):
                for i in range(i0, min(i0 + 8, NQ)):
                    c.op("pe", lambda: nc.tensor.transpose(tbank[:, (i - i0) * 128:(i - i0 + 1) * 128], pen[:, i, :], K.ident[:]),
                         reads=[bpen], writes=[btbank])
                nn = min(8, NQ - i0) * 128
                c.op("act", lambda: nc.scalar.copy(penT[0:16, i0 * 128:i0 * 128 + nn], tbank[:, 0:nn]), reads=[btbank], writes=[bpenT])
            os_, bos = osr.next()
            for i in range(NQ):
                own = i // 2
                ob, bob = obk.next()
                rb, brb = rbk.next()
                qs = q[:, i * 128:(i + 1) * 128]
                chunks = list(range(i + 1))
                first = True
                for c0 in range(0, len(chunks), 4):
                    grp = chunks[c0:c0 + 4]
                    sb_, bsb = sbk.next()
                    pt, bpt = ptr.next()
                    for jj, j in enumerate(grp):
                        past = (j // 2) < own
                        c.op("pe", lambda: nc.tensor.matmul(sb_[:, jj, :], k[:, j * 128:(j + 1) * 128], qs, start=True, stop=not past),
                             reads=[bk_, bq], writes=[bsb])
                        if past:
                            n = j // 2
                            c.op("pe", lambda: nc.tensor.matmul(sb_[:, jj, :], K.E[:, n * 128:(n + 1) * 128], penT[:, i * 128:(i + 1) * 128],
                                                                start=False, stop=True), reads=[bpenT], writes=[bsb])
                    for jj, j in enumerate(grp):
                        col = h * NQ + (i - j)
                        c.op("act", lambda: nc.scalar.activation(pt[:, jj, :], sb_[:, jj, :], AF.Exp, bias=K.alibi[:, col:col + 1], scale=scale),
                             reads=[bsb], writes=[bpt])
                        if j == i:
                            c.op("pool", lambda: nc.gpsimd.tensor_tensor(pt[:, jj, :], pt[:, jj, :], K.tri[:], ALU.mult),
                                 reads=[bpt], writes=[bpt])
                    for jj, j in enumerate(grp):
                        last = (j == i)
                        c.op("pe", lambda: nc.tensor.matmul(ob[:, 0:128], vt[:, j, :], pt[:, jj, :], start=first, stop=last),
                             reads=[bv, bpt], writes=[bob])
                        c.op("pe", lambda: nc.tensor.matmul(rb[:, 0:128], K.ones_b[:], pt[:, jj, :], start=first, stop=last),
                             reads=[bpt], writes=[brb])
                        first = False
                ri, bri = rir.next()
                c.op("dve", lambda: nc.vector.reciprocal(ri[:], rb[:, 0:128]), reads=[brb], writes=[bri])
                c.op("dve", lambda: nc.vector.tensor_tensor(os_[:, i * 128:(i + 1) * 128], ob[:, 0:128], ri[:], ALU.mult),
                     reads=[bob, bri], writes=[bos])
            c.dma(attnT[h * 128:(h + 1) * 128, :], os_[:], reads=[bos], sem_buf=bos)


def merge_phase(c, ya, yb, yc, wA, wB, wC, sg, mT, T):
    nc = c.nc
    TS = 512
    srcs = [y.rearrange("(k p) t -> p k t", p=128) for y in (ya, yb, yc)]
    ws = (wA, wB, wC)
    sgv = sg.rearrange("(b d) t -> d b t", b=3)
    with c.phase():
        act = Ring(c, "mg_a", [128, 3, 8, TS], BF16, 2)
        wt = Ring(c, "mg_w", [128, 3, 8, 256], BF16, 2)
        gr = Ring(c, "mg_g", [128, 3, 512], BF16, 3)
        tr = Ring(c, "mg_t", [128, 3, 512], F32, 2)
        mr = Ring(c, "mg_m", [128, 512], BF16, 3)
        banks = Ring(c, "mg_bk", [128, 512], F32, 6, psum=True)
        nsup = T // TS
        seq = [(su, s) for su in range(nsup) for s in range(8)]
        acts, wts = {}, {}

        def load_act(su):
            a, ba = act.next()
            for b in range(3):
                c.dma(a[:, b, :, :], srcs[b][:, :, su * TS:(su + 1) * TS], writes=[ba], sem_buf=ba)
            acts[su] = (a, ba)

        def load_w(i):
            w, bw = wt.next()
            for b in range(3):
                c.dma(w[:, b, :, :], ws[b][seq[i][1]], writes=[bw], sem_buf=bw)
            wts[i] = (w, bw)

        load_act(0)
        load_w(0)
        for i, (su, s) in enumerate(seq):
            if s == 0 and su + 1 < nsup:
                load_act(su + 1)
            if i + 1 < len(seq):
                load_w(i + 1)
            w, bw = wts.pop(i)
            a, ba = acts[su]
            t0 = su * TS
            for j in range(2):
                d0 = s * 256 + j * 128
                g, bg = gr.next()
                c.dma(g[:], sgv[d0:d0 + 128, :, t0:t0 + 512], writes=[bg], sem_buf=bg)
                bl = [banks.next() for _ in range(3)]
                for b in range(3):
                    for k in range(8):
                        c.op("pe", lambda: nc.tensor.matmul(bl[b][0][:], w[:, b, k, j * 128:(j + 1) * 128], a[:, b, k, :],
                                                            start=(k == 0), stop=(k == 7)), reads=[ba, bw], writes=[bl[b][1]])
                t, bt = tr.next()
                m, bm = mr.next()
                for b in range(3):
                    c.op("dve", lambda: nc.vector.tensor_tensor(t[:, b, :], bl[b][0][:], g[:, b, :], ALU.mult),
                         reads=[bl[b][1], bg], writes=[bt])
                c.op("pool", lambda: nc.gpsimd.tensor_tensor(t[:, 0, :], t[:, 0, :], t[:, 1, :], ALU.add), reads=[bt], writes=[bt])
                c.op("pool", lambda: nc.gpsimd.tensor_tensor(m[:], t[:, 0, :], t[:, 2, :], ALU.add), reads=[bt], writes=[bm])
                c.dma(mT[d0:d0 + 128, t0:t0 + 512], m[:], reads=[bm], sem_buf=bm)


def setup_consts(c, nc, K, T, din):
    NQ = T // 128
    gains_d = din("gains", [128, (3 * NL + 1) * 16])
    convw_d = din("convw", [128, NL * 8 * 3])
    pscale_d = din("pscale", [128, NL * 8])
    invd_d = din("invd", [128, 4 * 512])
    negmask_d = din("negmask", [128, NQ * 16])
    alibi_d = din("alibi", [128, NH * NQ])
    tri_d = din("tri", [128, 128])
    ident_d = din("ident", [128, 128])
    E_d = din("Emat", [128, 16 * 128])
    specs = [("gains", gains_d, [128, (3 * NL + 1) * 16], F32), ("convw", convw_d, [128, NL * 8 * 3], F32),
             ("pscale", pscale_d, [128, NL * 8], F32),
             ("invd", invd_d.rearrange("p (g t) -> p g t", g=4), [128, 4, 512], F32),
             ("negmask", negmask_d, [128, NQ * 16], F32), ("alibi", alibi_d, [128, NH * NQ], F32),
             ("tri", tri_d, [128, 128], BF16), ("ident", ident_d, [128, 128], BF16),
             ("E", E_d, [128, 16 * 128], BF16)]
    for (name, src, shape, dt) in specs:
        setattr(K, name, c.sbuf("k_" + name, shape, dt, persistent=True))
    K.ones_f = c.sbuf("k_ones_f", [128, 128], F32, persistent=True)
    K.ones_b = c.sbuf("k_ones_b", [128, 128], BF16, persistent=True)
    with c.phase():
        for (name, src, shape, dt) in specs:
            t = getattr(K, name)
            b = Buf("k_" + name)
            if dt == F32:
                c.dma(t[:], src, writes=[b], sem_buf=b)
            else:
                st = c.sbuf("ks_" + name, shape, F32)
                bs = Buf("ks_" + name)
                c.dma(st[:], src, writes=[bs], sem_buf=bs)
                c.op("dve", lambda: nc.vector.tensor_copy(t[:], st[:]), reads=[bs], writes=[b])
        c.op("dve", lambda: nc.vector.memset(K.ones_f[:], 1.0))
        c.op("dve", lambda: nc.vector.memset(K.ones_b[:], 1.0))


class Consts:
    pass


def build(T, stop_after=None, dbg=(), skip_ffn1=False):
    NQ = T // 128
    nc = bass.Bass("TRN2", target_bir_lowering=False)

    def din(name, shape):
        return nc.dram_tensor(name, list(shape), F32, kind="ExternalInput").ap()

    def scr(name, shape, dt):
        kind = "ExternalOutput" if name in dbg else "Internal"
        return nc.dram_tensor(name, list(shape), dt, kind=kind).ap()

    xT = din("xT", [D, T])
    W = {}
    W["ffn1_w_in"] = din("ffn1_w_in", [NL, D, 2 * DFF])
    W["ffn1_w_out"] = din("ffn1_w_out", [NL, DFF, D])
    W["mix_w_in"] = din("mix_w_in", [NL, D, INDIM])
    W["conv_w_out"] = din("conv_w_out", [NL, 1024, D])
    W["pool_w"] = din("pool_w", [NL, 4, 256, 256])
    W["pool_w_out"] = din("pool_w_out", [NL, 1024, D])
    W["attn_w_out"] = din("attn_w_out", [NL, 1024, D])
    W["mix_w_out"] = din("mix_w_out", [NL, D, D])
    W["ffn2_w_in"] = din("ffn2_w_in", [NL, D, 2 * DFF])
    W["ffn2_w_out"] = din("ffn2_w_out", [NL, DFF, D])
    yT = nc.dram_tensor("yT", [D, T], F32, kind="ExternalOutput").ap()

    S = {}
    for l in range(NL):
        S["f1i", l] = scr("s_f1i%d" % l, [22, 128, 16, 512], BF16)
        S["f1o", l] = scr("s_f1o%d" % l, [8, 128, 44, 256], BF16)
        S["mi", l] = scr("s_mi%d" % l, [26, 128, 16, 512], BF16)
        S["cwo", l] = scr("s_cwo%d" % l, [8, 128, 8, 256], BF16)
        S["pwo", l] = scr("s_pwo%d" % l, [8, 128, 8, 256], BF16)
        S["awo", l] = scr("s_awo%d" % l, [8, 128, 8, 256], BF16)
        S["mo", l] = scr("s_mo%d" % l, [8, 128, 16, 256], BF16)
        S["f2i", l] = scr("s_f2i%d" % l, [22, 128, 16, 512], BF16)
        S["f2o", l] = scr("s_f2o%d" % l, [8, 128, 44, 256], BF16)
    xres = scr("xres", [D, T], F32)
    hT = scr("hT", [D, T], BF16)
    gT = scr("gT", [DFF, T], BF16)
    pa = scr("pa", [4096, T], F32)
    qk = scr("qk", [2048, T], BF16)
    vS = scr("vS", [T, 1024], BF16)
    kbar = scr("kbar", [128, NH * 16], BF16)
    sgS = scr("sgS", [3 * D, T], BF16)
    ya = scr("ya", [1024, T], BF16)
    yb = scr("yb", [1024, T], BF16)
    yc = scr("yc", [1024, T], BF16)
    mT = scr("mT", [D, T], BF16)

    c = Ctx(nc)
    K = Consts()

    class Stop(Exception):
        pass

    def check(name):
        if stop_after == name:
            raise Stop()

    try:
        setup_consts(c, nc, K, T, din)
        check("consts")

        rot = ["dve", "pool", "act"]
        ffn_in_runs = [(0, 22, 256, 0, 0), (DFF, 22, 256, 0, 256)]
        for l in range(NL):
            if stop_after in ("ffn1", "norm1", "ffn1_in") and l > 0:
                continue
            if not skip_ffn1:
                cast_weight(c, W["ffn1_w_in"][l], D, ffn_in_runs, S["f1i", l], rot)
                cast_weight(c, W["ffn1_w_out"][l], DFF, [(0, 8, 256, 0, 0)], S["f1o", l], rot)
            if stop_after in ("ffn1", "norm1", "ffn1_in"):
                continue
            cast_weight(c, W["mix_w_in"][l], D, [(0, 26, 512, 0, 0)], S["mi", l], rot)
            cast_weight(c, W["conv_w_out"][l], 1024, [(0, 8, 256, 0, 0)], S["cwo", l], rot)
            cast_weight(c, W["pool_w_out"][l], 1024, [(0, 8, 256, 0, 0)], S["pwo", l], rot)
            cast_weight(c, W["attn_w_out"][l], 1024, [(0, 8, 256, 0, 0)], S["awo", l], rot)
            cast_weight(c, W["mix_w_out"][l], D, [(0, 8, 256, 0, 0)], S["mo", l], rot)
            if stop_after in ("mix", "mix_in", "conv", "pool", "attn", "merge") and l == 0:
                break
            cast_weight(c, W["ffn2_w_in"][l], D, ffn_in_runs, S["f2i", l], rot)
            cast_weight(c, W["ffn2_w_out"][l], DFF, [(0, 8, 256, 0, 0)], S["f2o", l], rot)
        check("cast")

        xcur = xT
        for l in range(NL):
            if not skip_ffn1:
                norm_phase(c, K, xcur, hT, BF16, K.gains[:, (l * 3 + 0) * 16:(l * 3 + 1) * 16], T)
                check("norm1")
                linear_phase(c, hT, D, S["f1i", l], 22, 512, 1024, T, EpiFfnIn(gT))
                check("ffn1_in")
                linear_phase(c, gT, DFF, S["f1o", l], 8, 256, 1024, T, EpiResid(xcur, xres, 0.5), nact=1)
                xcur = xres
                check("ffn1")
            norm_phase(c, K, xcur, hT, BF16, K.gains[:, (l * 3 + 1) * 16:(l * 3 + 2) * 16], T)
            linear_phase(c, hT, D, S["mi", l], 26, 512, 1024, T, EpiMixIn(pa, qk, vS, kbar, sgS, T), tokmajor=(12, 13))
            check("mix_in")
            conv_phase(c, K, l, pa, ya, T)
            check("conv")
            pool_phase(c, K, l, pa, W["pool_w"][l], yb, T)
            check("pool")
            attn_phase(c, K, qk, vS, kbar, yc, T)
            check("attn")
            merge_phase(c, ya, yb, yc, S["cwo", l], S["pwo", l], S["awo", l], sgS, mT, T)
            check("merge")
            linear_phase(c, mT, D, S["mo", l], 8, 256, 1024, T, EpiResid(xcur, xres, 1.0))
            xcur = xres
            check("mix")
            norm_phase(c, K, xres, hT, BF16, K.gains[:, (l * 3 + 2) * 16:(l * 3 + 3) * 16], T)
            linear_phase(c, hT, D, S["f2i", l], 22, 512, 1024, T, EpiFfnIn(gT))
            linear_phase(c, gT, DFF, S["f2o", l], 8, 256, 1024, T, EpiResid(xres, xres, 0.5), nact=1)
            check("layer%d" % l)
        norm_phase(c, K, xres, yT, F32, K.gains[:, 3 * NL * 16:(3 * NL + 1) * 16], T)
    except Stop:
        pass
    c.barrier()
    c.es.close()
    return nc, c


def host_consts(T):
    NQ = T // 128
    k = {}
    t = np.arange(512, dtype=np.float32)
    invd = np.stack([1.0 / np.minimum(t + 1.0, float(w)) for w in (2, 4, 8, 16)], 0)
    k["invd"] = np.ascontiguousarray(np.broadcast_to(invd.reshape(1, 4 * 512), (128, 4 * 512))).astype(np.float32)
    nm = np.zeros((NQ, 16), np.float32)
    for i in range(NQ):
        nm[i, (i // 2):] = NEG
    k["negmask"] = np.ascontiguousarray(np.broadcast_to(nm.reshape(1, NQ * 16), (128, NQ * 16))).astype(np.float32)
    slopes = np.exp2(-(8.0 / NH) * np.arange(1, NH + 1, dtype=np.float64))
    ki = np.arange(128, dtype=np.float64)[:, None, None]
    rel = np.arange(NQ, dtype=np.float64)[None, None, :]
    al = slopes[None, :, None] * (ki - rel * 128.0 - 64.0)
    k["alibi"] = np.ascontiguousarray(al.reshape(128, NH * NQ)).astype(np.float32)
    kk = np.arange(128)
    k["tri"] = (kk[:, None] <= kk[None, :]).astype(np.float32)
    k["ident"] = np.eye(128, dtype=np.float32)
    E = np.zeros((128, 16, 128), np.float32)
    for n in range(16):
        E[n, n, :] = 1.0
    k["Emat"] = np.ascontiguousarray(E.reshape(128, 16 * 128))
    return k


def chunked(vec):
    return np.ascontiguousarray(np.asarray(vec, np.float32).reshape(-1, 128).T)


def host_small(inp):
    g = []
    for l in range(NL):
        g += [chunked(inp["ffn1_norm"][l]), chunked(inp["mix_norm"][l]), chunked(inp["ffn2_norm"][l])]
    g.append(chunked(inp["final_norm"]))
    out = {"gains": np.ascontiguousarray(np.concatenate(g, axis=1))}
    cw = np.asarray(inp["conv_w"], np.float32)
    cw = cw.reshape(NL, 3, 8, 128).transpose(3, 0, 2, 1)
    out["convw"] = np.ascontiguousarray(cw.reshape(128, NL * 8 * 3))
    ps = np.asarray(inp["pool_scale"], np.float32).reshape(NL, 8, 128).transpose(2, 0, 1)
    out["pscale"] = np.ascontiguousarray(ps.reshape(128, NL * 8))
    return out


WNAMES = ["ffn1_w_in", "ffn1_w_out", "mix_w_in", "conv_w_out", "pool_w", "pool_w_out", "attn_w_out",
          "mix_w_out", "ffn2_w_in", "ffn2_w_out"]


def kernel(**inputs):
    x = np.asarray(inputs["x"], np.float32)
    B, T, _ = x.shape
    nc, _ = build(T)
    base = {n: np.ascontiguousarray(np.asarray(inputs[n], np.float32)) for n in WNAMES}
    base.update(host_consts(T))
    base.update(host_small(inputs))
    in_maps = []
    for b in range(B):
        m = dict(base)
        m["xT"] = np.ascontiguousarray(x[b].T)
        in_maps.append(m)
    res = run_bass_kernel_spmd(nc, in_maps, core_ids=list(range(B)))
    out = np.stack([np.ascontiguousarray(res.results[b]["yT"].T) for b in range(B)], 0)
    return out.astype(np.float32)
```

```python
from contextlib import ExitStack, contextmanager
import numpy as np
import concourse.bass as bass
import concourse.mybir as mybir
from concourse.bass_utils import run_bass_kernel_spmd

F32 = mybir.dt.float32
BF16 = mybir.dt.bfloat16
AF = mybir.ActivationFunctionType
ALU = mybir.AluOpType
AX = mybir.AxisListType

D = 2048
DFF = 5632
NL = 2
INDIM = 13312
NH = 8
EPS = 1e-6
NEG = -1.0e30
PEN = -30000.0
DBG = {}


class Buf:
    __slots__ = ("name", "w", "r", "dsem", "persist")

    def __init__(self, name, persist=False):
        self.name = name
        self.w = None
        self.r = {}
        self.dsem = None
        self.persist = persist


class SemSlot:
    __slots__ = ("key", "cnt")

    def __init__(self, key):
        self.key = key
        self.cnt = 0


class Ctx:
    NDSEM = 56

    def __init__(self, nc):
        self.nc = nc
        self.es = ExitStack()
        self.pes = None
        self.eng = {"pe": nc.tensor, "act": nc.scalar, "dve": nc.vector,
                    "pool": nc.gpsimd, "sp": nc.sync}
        self.semh = {}
        self.cnt = {}
        for k in self.eng:
            self.semh[k] = self.es.enter_context(nc.semaphore("s_" + k))
            self.cnt[k] = 0
        self.known = {k: {} for k in self.eng}
        self.slots = []
        for i in range(self.NDSEM):
            key = "d%d" % i
            self.semh[key] = self.es.enter_context(nc.semaphore(key))
            self.slots.append(SemSlot(key))
        self.free_slots = list(self.slots)
        self.phase_slots = []
        self.nwaits = 0
        self.ninstr = 0
        self.uid = 0
        self.bg = None

    def sbuf(self, name, shape, dt, persistent=False):
        self.uid += 1
        st = self.es if (persistent or self.pes is None) else self.pes
        return st.enter_context(self.nc.sbuf_tensor("%s_%d" % (name, self.uid), list(shape), dt))

    def psum(self, name, shape, dt):
        self.uid += 1
        return self.pes.enter_context(self.nc.psum_tensor("%s_%d" % (name, self.uid), list(shape), dt))

    def _wait(self, E, deps):
        eng = self.eng[E]
        kn = self.known[E]
        best = {}
        for d in deps:
            if d is None:
                continue
            k, v = d
            if E == "pe" and k == "pe":
                continue
            if best.get(k, 0) < v:
                best[k] = v
        for k, v in best.items():
            if kn.get(k, 0) < v:
                eng.wait_ge(self.semh[k], v)
                kn[k] = v
                self.nwaits += 1

    @staticmethod
    def _deps(reads, writes):
        deps = []
        for b in reads:
            deps.append(b.w)
        for b in writes:
            deps.append(b.w)
            deps.extend(b.r.items())
        return deps

    def op(self, E, fn, reads=(), writes=()):
        self._wait(E, self._deps(reads, writes))
        ins = fn()
        self.cnt[E] += 1
        cn = self.cnt[E]
        ins.then_inc(self.semh[E], 1)
        self.ninstr += 1
        for b in reads:
            if b.r.get(E, 0) < cn:
                b.r[E] = cn
        for b in writes:
            b.w = (E, cn)
            b.r = {}
        return ins

    def dma(self, out_ap, in_ap, reads=(), writes=(), sem_buf=None, Q="sp", **kw):
        self._wait(Q, self._deps(reads, writes))
        sb = sem_buf
        if sb.dsem is None:
            sb.dsem = self.free_slots.pop()
            if not sb.persist:
                self.phase_slots.append(sb.dsem)
        ins = self.eng[Q].dma_start(out=out_ap, in_=in_ap, **kw)
        sb.dsem.cnt += 16
        ins.then_inc(self.semh[sb.dsem.key], 16)
        self.ninstr += 1
        ev = (sb.dsem.key, sb.dsem.cnt)
        for b in reads:
            if b.r.get(ev[0], 0) < ev[1]:
                b.r[ev[0]] = ev[1]
        for b in writes:
            b.w = ev
            b.r = {}
        return ins

    def barrier(self):
        allev = [(k, v) for k, v in self.cnt.items() if v > 0]
        allev += [(s.key, s.cnt) for s in self.slots if s.cnt > 0]
        for E in self.eng:
            self._wait(E, allev)

    @contextmanager
    def phase(self):
        self.pes = ExitStack()
        try:
            yield
            self.barrier()
        finally:
            self.pes.close()
            self.pes = None
            self.free_slots.extend(self.phase_slots)
            self.phase_slots = []


class Ring:
    def __init__(self, c, name, shape, dt, n, psum=False):
        self.t = []
        self.b = []
        for i in range(n):
            nm = "%s%d" % (name, i)
            self.t.append(c.psum(nm, shape, dt) if psum else c.sbuf(nm, shape, dt))
            self.b.append(Buf(nm))
        self.i = 0
        self.n = n

    def next(self):
        i = self.i % self.n
        self.i += 1
        return self.t[i], self.b[i]


class BgCast:
    MAXC = 1664

    def __init__(self, c):
        self.c = c
        n = 3
        self.tin = [c.sbuf("bg_in%d" % i, [128, self.MAXC], F32, persistent=True) for i in range(n)]
        self.tout = [c.sbuf("bg_out%d" % i, [128, self.MAXC], BF16, persistent=True) for i in range(n)]
        self.bin = [Buf("bg_in%d" % i, persist=True) for i in range(n)]
        self.bout = [Buf("bg_out%d" % i, persist=True) for i in range(n)]
        self.n = n
        self.items = []
        self.pos = 0
        self.left = {}
        self.loaded = []
        self.casted = []
        self.k = 0

    def add(self, key, src, K, runs, dst):
        KC = K // 128
        self.left.setdefault(key, 0)
        for kc in range(KC):
            for (col0, nseg, w, s0, c0) in runs:
                per = max(1, self.MAXC // w)
                sg = 0
                while sg < nseg:
                    n = min(per, nseg - sg)
                    a = col0 + sg * w
                    sv = src[kc * 128:(kc + 1) * 128, a:a + n * w]
                    dv = dst[s0 + sg:s0 + sg + n, :, kc, c0:c0 + w].rearrange("s p c -> p s c")
                    self.items.append((key, sv, dv, n, w))
                    self.left[key] += 1
                    sg += n

    def _store(self):
        c = self.c
        key, dv, n, w, i = self.casted.pop(0)
        c.dma(dv, self.tout[i][:, 0:n * w].rearrange("p (s c) -> p s c", c=w), reads=[self.bout[i]], sem_buf=self.bout[i])
        self.left[key] -= 1

    def _cast(self):
        c = self.c
        nc = c.nc
        key, dv, n, w, i = self.loaded.pop(0)
        c.op("pool", lambda: nc.gpsimd.tensor_copy(self.tout[i][:, 0:n * w], self.tin[i][:, 0:n * w]),
             reads=[self.bin[i]], writes=[self.bout[i]])
        self.casted.append((key, dv, n, w, i))

    def _load(self):
        c = self.c
        key, sv, dv, n, w = self.items[self.pos]
        self.pos += 1
        i = self.k % self.n
        self.k += 1
        c.dma(self.tin[i][:, 0:n * w], sv, writes=[self.bin[i]], sem_buf=self.bin[i])
        self.loaded.append((key, dv, n, w, i))

    def step(self, n=1):
        for _ in range(n):
            if self.casted:
                self._store()
            if self.loaded:
                self._cast()
            if self.pos < len(self.items):
                self._load()

    def pending(self):
        return self.pos < len(self.items) or self.loaded or self.casted

    def ensure(self, key):
        while self.left.get(key, 0) > 0:
            self.step()


def cast_weight(c, src, K, runs, dst, eng_rot):
    nc = c.nc
    KC = K // 128
    with c.phase():
        maxc = 3328
        fin = Ring(c, "cw_in", [128, maxc], F32, 3)
        fout = Ring(c, "cw_out", [128, maxc], BF16, 3)
        for kc in range(KC):
            for (col0, nseg, w, s0, c0) in runs:
                per = max(1, maxc // w)
                sg = 0
                while sg < nseg:
                    n = min(per, nseg - sg)
                    ncols = n * w
                    a = col0 + sg * w
                    ti, bi = fin.next()
                    to, bo = fout.next()
                    c.dma(ti[:, 0:ncols], src[kc * 128:(kc + 1) * 128, a:a + ncols], writes=[bi], sem_buf=bi)
                    E = eng_rot[0]
                    eng_rot.append(eng_rot.pop(0))
                    if E == "act":
                        c.op("act", lambda: nc.scalar.copy(to[:, 0:ncols], ti[:, 0:ncols]), reads=[bi], writes=[bo])
                    elif E == "dve":
                        c.op("dve", lambda: nc.vector.tensor_copy(to[:, 0:ncols], ti[:, 0:ncols]), reads=[bi], writes=[bo])
                    else:
                        c.op("pool", lambda: nc.gpsimd.tensor_copy(to[:, 0:ncols], ti[:, 0:ncols]), reads=[bi], writes=[bo])
                    dv = dst[s0 + sg:s0 + sg + n, :, kc, c0:c0 + w].rearrange("s p c -> p s c")
                    c.dma(dv, to[:, 0:ncols].rearrange("p (s c) -> p s c", c=w), reads=[bo], sem_buf=bo)
                    sg += n


def norm_phase(c, K, src, dst, dst_dt, gain_ap, T):
    nc = c.nc
    TT = 256
    srcv = src.rearrange("(k p) t -> p k t", p=128)
    dstv = dst.rearrange("(k p) t -> p k t", p=128)
    with c.phase():
        xr = Ring(c, "nx", [128, 16, TT], F32, 2)
        sr = Ring(c, "nsq", [128, 16, TT], F32, 2)
        hr = Ring(c, "nh", [128, 16, TT], dst_dt, 2)
        rr = Ring(c, "nr", [128, TT], F32, 2)
        pr = Ring(c, "nps", [128, 512], F32, 2, psum=True)
        for tt in range(T // TT):
            x, bx = xr.next()
            sq, bs = sr.next()
            h, bh = hr.next()
            r, br = rr.next()
            ps, bp = pr.next()
            c.dma(x[:], srcv[:, :, tt * TT:(tt + 1) * TT], writes=[bx], sem_buf=bx)
            c.op("act", lambda: nc.scalar.activation(sq[:], x[:], AF.Square), reads=[bx], writes=[bs])
            for k in range(16):
                c.op("pe", lambda: nc.tensor.matmul(ps[:, 0:TT], K.ones_f[:], sq[:, k, :], start=(k == 0), stop=(k == 15)),
                     reads=[bs], writes=[bp])
            c.op("dve", lambda: nc.vector.tensor_scalar(r[:], ps[:, 0:TT], 1.0 / D, EPS, ALU.mult, ALU.add),
                 reads=[bp], writes=[br])
            c.op("act", lambda: nc.scalar.activation(r[:], r[:], AF.Sqrt), reads=[br], writes=[br])
            c.op("dve", lambda: nc.vector.reciprocal(r[:], r[:]), reads=[br], writes=[br])
            for k in range(16):
                c.op("dve", lambda: nc.vector.scalar_tensor_tensor(h[:, k, :], x[:, k, :], gain_ap[:, k:k + 1], r[:], ALU.mult, ALU.mult),
                     reads=[bx, br], writes=[bh])
            c.dma(dstv[:, :, tt * TT:(tt + 1) * TT], h[:], reads=[bh], sem_buf=bh)


def linear_phase(c, srcT, Kdim, wsl, S, C, TS, T, epi, nact=2, tokmajor=()):
    nc = c.nc
    KC = Kdim // 128
    G = C // 128
    nsup = T // TS
    nsub = TS // 512
    srcv = srcT.rearrange("(k p) t -> p k t", p=128)
    with c.phase():
        act = Ring(c, "act", [128, KC, TS], BF16, nact)
        wt = Ring(c, "wt", [128, KC, C], BF16, 2)
        banks = Ring(c, "bk", [128, 512], F32, 8, psum=True)
        epi.setup(c)
        seq = [(su, s) for su in range(nsup) for s in range(S)]
        acts = {}

        def load_act(su):
            a, ba = act.next()
            for k0 in range(0, KC, 8):
                k1 = min(KC, k0 + 8)
                c.dma(a[:, k0:k1, :], srcv[:, k0:k1, su * TS:(su + 1) * TS], writes=[ba], sem_buf=ba)
            acts[su] = (a, ba)

        wts = {}

        def load_w(i):
            w, bw = wt.next()
            c.dma(w[:], wsl[seq[i][1]], writes=[bw], sem_buf=bw)
            wts[i] = (w, bw)

        load_act(0)
        load_w(0)
        for i, (su, s) in enumerate(seq):
            if s == 0 and su + 1 < nsup and nact > 1:
                load_act(su + 1)
            if su not in acts:
                load_act(su)
            if i + 1 < len(seq):
                load_w(i + 1)
            w, bw = wts.pop(i)
            a, ba = acts[su]
            if c.bg is not None:
                c.bg.step(c.bg_rate)
            for sb in range(nsub):
                t0 = su * TS + sb * 512
                bl = [banks.next() for _ in range(4 if s in tokmajor else G)]
                if s in tokmajor:
                    for g in range(4):
                        for k in range(KC):
                            c.op("pe", lambda: nc.tensor.matmul(bl[g][0][:, 0:C], a[:, k, sb * 512 + g * 128:sb * 512 + (g + 1) * 128],
                                                                w[:, k, :], start=(k == 0), stop=(k == KC - 1)),
                                 reads=[ba, bw], writes=[bl[g][1]])
                else:
                    for g in range(G):
                        for k in range(KC):
                            c.op("pe", lambda: nc.tensor.matmul(bl[g][0][:], w[:, k, g * 128:(g + 1) * 128],
                                                                a[:, k, sb * 512:(sb + 1) * 512], start=(k == 0), stop=(k == KC - 1)),
                                 reads=[ba, bw], writes=[bl[g][1]])
                epi(c, s, t0, [b[0] for b in bl], [b[1] for b in bl])
        epi.finish(c)


class EpiFfnIn:
    def __init__(self, gT):
        self.gT = gT

    def setup(self, c):
        self.sa = Ring(c, "e_sa", [128, 512], F32, 4)
        self.go = Ring(c, "e_go", [128, 2, 512], BF16, 3)

    def __call__(self, c, s, t0, bk, bb):
        nc = c.nc
        go, bgo = self.go.next()
        for j in range(2):
            sa, bsa = self.sa.next()
            c.op("act", lambda: nc.scalar.activation(sa[:], bk[j][:], AF.Silu), reads=[bb[j]], writes=[bsa])
            c.op("dve", lambda: nc.vector.tensor_tensor(go[:, j, :], sa[:], bk[2 + j][:], ALU.mult),
                 reads=[bsa, bb[2 + j]], writes=[bgo])
        dv = self.gT[s * 256:(s + 1) * 256, t0:t0 + 512].rearrange("(j p) t -> p j t", p=128)
        c.dma(dv, go[:], reads=[bgo], sem_buf=bgo)

    def finish(self, c):
        pass


class EpiResid:
    def __init__(self, xsrc, xdst, coef):
        self.xsrc, self.xdst, self.coef = xsrc, xdst, coef

    def setup(self, c):
        self.xr = Ring(c, "e_x", [128, 2, 512], F32, 4)

    def __call__(self, c, s, t0, bk, bb):
        nc = c.nc
        x, bx = self.xr.next()
        sv = self.xsrc[s * 256:(s + 1) * 256, t0:t0 + 512].rearrange("(j p) t -> p j t", p=128)
        dv = self.xdst[s * 256:(s + 1) * 256, t0:t0 + 512].rearrange("(j p) t -> p j t", p=128)
        c.dma(x[:], sv, writes=[bx], sem_buf=bx)
        for j in range(2):
            c.op("dve", lambda: nc.vector.scalar_tensor_tensor(x[:, j, :], bk[j][:], float(self.coef), x[:, j, :], ALU.mult, ALU.add),
                 reads=[bb[j], bx], writes=[bx])
        c.dma(dv, x[:], reads=[bx], sem_buf=bx)

    def finish(self, c):
        pass


class EpiMixIn:
    def __init__(self, pa, qk, v, kbar, sg, T):
        self.pa, self.qk, self.v, self.kbar, self.sg, self.T = pa, qk, v, kbar, sg, T

    def setup(self, c):
        self.f = Ring(c, "e_f", [128, 4, 512], F32, 2)
        self.h = Ring(c, "e_h", [128, 4, 512], BF16, 3)
        self.kf = Ring(c, "e_kf", [128, 512], F32, 2)
        self.kb = c.sbuf("e_kb", [128, 8, self.T // 256], F32)
        self.bkb = Buf("e_kb")
        self.kbo = c.sbuf("e_kbo", [128, 8, 16], BF16)
        self.bkbo = Buf("e_kbo")
        self.n = 0

    def __call__(self, c, s, t0, bk, bb):
        nc = c.nc
        self.n += 1
        if s < 8:
            f, bf = self.f.next()
            for j in range(4):
                if (self.n + j) % 2 == 0:
                    c.op("act", lambda: nc.scalar.copy(f[:, j, :], bk[j][:]), reads=[bb[j]], writes=[bf])
                else:
                    c.op("dve", lambda: nc.vector.tensor_copy(f[:, j, :], bk[j][:]), reads=[bb[j]], writes=[bf])
            dv = self.pa[s * 512:(s + 1) * 512, t0:t0 + 512].rearrange("(j p) t -> p j t", p=128)
            c.dma(dv, f[:], reads=[bf], sem_buf=bf)
        elif s < 12:
            h, bh = self.h.next()
            for j in range(4):
                if s < 10:
                    if (self.n + j) % 2 == 0:
                        c.op("act", lambda: nc.scalar.copy(h[:, j, :], bk[j][:]), reads=[bb[j]], writes=[bh])
                    else:
                        c.op("dve", lambda: nc.vector.tensor_copy(h[:, j, :], bk[j][:]), reads=[bb[j]], writes=[bh])
                else:
                    hd = (s - 10) * 4 + j
                    b0 = t0 // 256
                    kf, bkf = self.kf.next()
                    c.op("act", lambda: nc.scalar.copy(kf[:], bk[j][:]), reads=[bb[j]], writes=[bkf])
                    c.op("pool", lambda: nc.gpsimd.tensor_copy(h[:, j, :], kf[:]), reads=[bkf], writes=[bh])
                    for hb in range(2):
                        c.op("dve", lambda: nc.vector.reduce_sum(self.kb[:, hd, b0 + hb:b0 + hb + 1], kf[:, hb * 256:(hb + 1) * 256], AX.X),
                             reads=[bkf], writes=[self.bkb])
            r0 = (s - 8) * 512
            dv = self.qk[r0:r0 + 512, t0:t0 + 512].rearrange("(j p) t -> p j t", p=128)
            c.dma(dv, h[:], reads=[bh], sem_buf=bh)
        elif s < 14:
            h, bh = self.h.next()
            for g in range(4):
                if (self.n + g) % 2 == 0:
                    c.op("act", lambda: nc.scalar.copy(h[:, g, :], bk[g][:]), reads=[bb[g]], writes=[bh])
                else:
                    c.op("dve", lambda: nc.vector.tensor_copy(h[:, g, :], bk[g][:]), reads=[bb[g]], writes=[bh])
            c0 = (s - 12) * 512
            dv = self.v[t0:t0 + 512, c0:c0 + 512].rearrange("(g p) f -> p g f", p=128)
            c.dma(dv, h[:], reads=[bh], sem_buf=bh)
        else:
            h, bh = self.h.next()
            for j in range(4):
                c.op("act", lambda: nc.scalar.activation(h[:, j, :], bk[j][:], AF.Sigmoid), reads=[bb[j]], writes=[bh])
            r0 = (s - 14) * 512
            dv = self.sg[r0:r0 + 512, t0:t0 + 512].rearrange("(j p) t -> p j t", p=128)
            c.dma(dv, h[:], reads=[bh], sem_buf=bh)

    def finish(self, c):
        nc = c.nc
        NB = self.T // 256
        c.op("dve", lambda: nc.vector.memset(self.kbo[:], 0.0), writes=[self.bkbo])
        c.op("dve", lambda: nc.vector.tensor_scalar(self.kbo[:, :, 0:NB], self.kb[:], 1.0 / 256, None, ALU.mult),
             reads=[self.bkb], writes=[self.bkbo])
        c.dma(self.kbar.rearrange("p (h n) -> p h n", n=16), self.kbo[:], reads=[self.bkbo], sem_buf=self.bkbo)


def conv_phase(c, K, l, pa, ya, T):
    nc = c.nc
    TT = 1024
    cxv = pa[1024:3072, :].rearrange("(two c) t -> c two t", two=2)
    with c.phase():
        cxr = Ring(c, "cv_cx", [128, 2, TT + 2], F32, 2)
        btr = Ring(c, "cv_b", [128, TT], F32, 2)
        ur = Ring(c, "cv_u", [128, TT + 2], F32, 2)
        ar = Ring(c, "cv_a", [128, TT], F32, 2)
        yr = Ring(c, "cv_y", [128, TT], BF16, 2)
        for ch in range(8):
            wb = (l * 8 + ch) * 3
            for tt in range(T // TT):
                t0 = tt * TT
                cx, bcx = cxr.next()
                bt, bbt = btr.next()
                u, bu = ur.next()
                a, ba = ar.next()
                y, by = yr.next()
                if t0 == 0:
                    c.op("pool", lambda: nc.gpsimd.memset(cx[:, :, 0:2], 0.0), writes=[bcx])
                    c.dma(cx[:, :, 2:], cxv[ch * 128:(ch + 1) * 128, :, 0:TT], writes=[bcx], sem_buf=bcx)
                else:
                    c.dma(cx[:], cxv[ch * 128:(ch + 1) * 128, :, t0 - 2:t0 + TT], writes=[bcx], sem_buf=bcx)
                c.dma(bt[:], pa[ch * 128:(ch + 1) * 128, t0:t0 + TT], writes=[bbt], sem_buf=bbt)
                c.op("pool", lambda: nc.gpsimd.tensor_tensor(u[:], cx[:, 0, :], cx[:, 1, :], ALU.mult), reads=[bcx], writes=[bu])
                c.op("dve", lambda: nc.vector.tensor_scalar(a[:], u[:, 2:TT + 2], K.convw[:, wb + 2:wb + 3], None, ALU.mult),
                     reads=[bu], writes=[ba])
                c.op("dve", lambda: nc.vector.scalar_tensor_tensor(a[:], u[:, 1:TT + 1], K.convw[:, wb + 1:wb + 2], a[:], ALU.mult, ALU.add),
                     reads=[bu, ba], writes=[ba])
                c.op("dve", lambda: nc.vector.scalar_tensor_tensor(a[:], u[:, 0:TT], K.convw[:, wb:wb + 1], a[:], ALU.mult, ALU.add),
                     reads=[bu, ba], writes=[ba])
                c.op("pool", lambda: nc.gpsimd.tensor_tensor(y[:], bt[:], a[:], ALU.mult), reads=[bbt, ba], writes=[by])
                c.dma(ya[ch * 128:(ch + 1) * 128, t0:t0 + TT], y[:], reads=[by], sem_buf=by)


def pool_phase(c, K, l, pa, pool_w_l, yb, T):
    nc = c.nc
    TT = 512
    H = 16
    with c.phase():
        pwf = c.sbuf("pl_wf", [128, 4, 2, 256], F32)
        pwb = c.sbuf("pl_wb", [128, 4, 2, 256], BF16)
        bpwf, bpwb = Buf("pl_wf"), Buf("pl_wb")
        c.dma(pwf[:], pool_w_l.rearrange("g (k p) d -> p g k d", p=128), writes=[bpwf], sem_buf=bpwf)
        c.op("dve", lambda: nc.vector.tensor_copy(pwb[:], pwf[:]), reads=[bpwf], writes=[bpwb])
        pr = Ring(c, "pl_p", [128, 2, TT + H], F32, 2)
        s1r = Ring(c, "pl_s1", [128, 2, TT + H], F32, 2)
        s2r = Ring(c, "pl_s2", [128, 2, TT + H], F32, 2)
        dr = Ring(c, "pl_d", [128, 2, TT], BF16, 2)
        yr = Ring(c, "pl_y", [128, 2, TT], BF16, 2)
        banks = Ring(c, "pl_bk", [128, 512], F32, 4, psum=True)
        for g in range(4):
            w = 2 << g
            pv = pa[3072 + g * 256:3072 + (g + 1) * 256, :].rearrange("(ci p) t -> p ci t", p=128)
            for tt in range(T // TT):
                t0 = tt * TT
                p, bp = pr.next()
                s1, bs1 = s1r.next()
                s2, bs2 = s2r.next()
                d, bd = dr.next()
                y, by = yr.next()
                if t0 == 0:
                    c.op("pool", lambda: nc.gpsimd.memset(p[:, :, 0:H], 0.0), writes=[bp])
                    c.dma(p[:, :, H:], pv[:, :, 0:TT], writes=[bp], sem_buf=bp)
                else:
                    c.dma(p[:], pv[:, :, t0 - H:t0 + TT], writes=[bp], sem_buf=bp)
                cur, bcur = p, bp
                sh = 1
                tog = 0
                while sh < w:
                    nxt, bnxt = (s1, bs1) if tog == 0 else (s2, bs2)
                    tog ^= 1
                    E = "dve" if tog else "pool"
                    eng = nc.vector if E == "dve" else nc.gpsimd
                    c.op(E, lambda: eng.tensor_tensor(nxt[:, :, sh:], cur[:, :, sh:], cur[:, :, 0:TT + H - sh], ALU.add),
                         reads=[bcur], writes=[bnxt])
                    cur, bcur = nxt, bnxt
                    sh *= 2
                if t0 == 0:
                    for ci in range(2):
                        c.op("dve", lambda: nc.vector.tensor_tensor(cur[:, ci, H:], cur[:, ci, H:], K.invd[:, g, :], ALU.mult),
                             reads=[bcur], writes=[bcur])
                    c.op("dve", lambda: nc.vector.tensor_tensor(d[:], cur[:, :, H:], p[:, :, H:], ALU.subtract),
                         reads=[bcur, bp], writes=[bd])
                else:
                    c.op("dve", lambda: nc.vector.scalar_tensor_tensor(d[:], cur[:, :, H:], 1.0 / w, p[:, :, H:], ALU.mult, ALU.subtract),
                         reads=[bcur, bp], writes=[bd])
                for j in range(2):
                    bk, bbk = banks.next()
                    for ci in range(2):
                        c.op("pe", lambda: nc.tensor.matmul(bk[:], pwb[:, g, ci, j * 128:(j + 1) * 128], d[:, ci, :],
                                                            start=(ci == 0), stop=(ci == 1)),
                             reads=[bpwb, bd], writes=[bbk])
                    sc = l * 8 + g * 2 + j
                    c.op("act", lambda: nc.scalar.activation(y[:, j, :], bk[:], AF.Copy, scale=K.pscale[:, sc:sc + 1]),
                         reads=[bbk], writes=[by])
                dv = yb[g * 256:(g + 1) * 256, t0:t0 + TT].rearrange("(j p) t -> p j t", p=128)
                c.dma(dv, y[:], reads=[by], sem_buf=by)


def attn_phase(c, K, qk, v, kbar, attnT, T):
    nc = c.nc
    NQ = T // 128
    scale = 128.0 ** -0.5
    vv = v.rearrange("(c p) f -> p c f", p=128)
    with c.phase():
        qr = Ring(c, "at_q", [128, T], BF16, 2)
        kr = Ring(c, "at_k", [128, T], BF16, 2)
        vr = Ring(c, "at_v", [128, NQ, 128], BF16, 2)
        kbr = Ring(c, "at_kb", [128, 16], BF16, 2)
        gm = c.sbuf("at_gm", [128, NQ, 16], F32)
        bgm = Buf("at_gm")
        top = c.sbuf("at_top", [128, NQ, 8], F32)
        btop = Buf("at_top")
        thr = c.sbuf("at_thr", [128, NQ], F32)
        bthr = Buf("at_thr")
        pen = c.sbuf("at_pen", [128, NQ, 16], BF16)
        bpen = Buf("at_pen")
        penT = c.sbuf("at_penT", [128, T], BF16)
        bpenT = Buf("at_penT")
        c.op("pool", lambda: nc.gpsimd.memset(penT[:], 0.0), writes=[bpenT])
        ptr = Ring(c, "at_pt", [128, 4, 128], BF16, 3)
        rir = Ring(c, "at_ri", [128, 128], F32, 2)
        osr = Ring(c, "at_os", [128, T], BF16, 2)
        gbank = c.psum("at_gb", [128, 512], F32)
        bgbank = Buf("at_gb")
        tbank = c.psum("at_tb", [16, 1024], BF16)
        btbank = Buf("at_tb")
        sbk = Ring(c, "at_s", [128, 4, 128], F32, 2, psum=True)
        obk = Ring(c, "at_o", [128, 512], F32, 2, psum=True)
        rbk = Ring(c, "at_r", [128, 512], F32, 2, psum=True)

        heads = {}

        def load_head(h):
            q, bq = qr.next()
            k, bk_ = kr.next()
            vt, bv = vr.next()
            kb, bkb = kbr.next()
            c.dma(q[:], qk[h * 128:(h + 1) * 128, :], writes=[bq], sem_buf=bq)
            c.dma(k[:], qk[1024 + h * 128:1024 + (h + 1) * 128, :], writes=[bk_], sem_buf=bk_)
            for c0 in range(0, NQ, 8):
                c.dma(vt[:, c0:c0 + 8, :], vv[:, c0:c0 + 8, h * 128:(h + 1) * 128], writes=[bv], sem_buf=bv)
            c.dma(kb[:], kbar[:, h * 16:(h + 1) * 16], writes=[bkb], sem_buf=bkb)
            heads[h] = (q, bq, k, bk_, vt, bv, kb, bkb)

        load_head(0)
        for h in range(NH):
            if h + 1 < NH:
                load_head(h + 1)
            q, bq, k, bk_, vt, bv, kb, bkb = heads.pop(h)
            for i in range(NQ):
                c.op("pe", lambda: nc.tensor.matmul(gbank[:, i * 16:(i + 1) * 16], q[:, i * 128:(i + 1) * 128], kb[:],
                                                    start=True, stop=True), reads=[bq, bkb], writes=[bgbank])
            c.op("dve", lambda: nc.vector.tensor_tensor(gm[:].rearrange("p a b -> p (a b)"), gbank[:, 0:NQ * 16], K.negmask[:], ALU.add),
                 reads=[bgbank], writes=[bgm])
            for i in range(NQ):
                c.op("dve", lambda: nc.vector.max(out=top[:, i, :], in_=gm[:, i, :]), reads=[bgm], writes=[btop])
            c.op("dve", lambda: nc.vector.tensor_scalar(thr[:], top[:, :, 2], -1.0e29, None, ALU.max), reads=[btop], writes=[bthr])
            for i in range(NQ):
                c.op("dve", lambda: nc.vector.tensor_scalar(pen[:, i, :], gm[:, i, :], thr[:, i:i + 1], PEN, ALU.is_lt, ALU.mult),
                     reads=[bgm, bthr], writes=[bpen])
            for i0 in range(0, NQ, 8):
                for i in range(i0, min(i0 + 8, NQ)):
                    c.op("pe", lambda: nc.tensor.transpose(tbank[:, (i - i0) * 128:(i - i0 + 1) * 128], pen[:, i, :], K.ident[:]),
                         reads=[bpen], writes=[btbank])
                nn = min(8, NQ - i0) * 128
                c.op("act", lambda: nc.scalar.copy(penT[0:16, i0 * 128:i0 * 128 + nn], tbank[:, 0:nn]), reads=[btbank], writes=[bpenT])
            os_, bos = osr.next()
            for i in range(NQ):
                own = i // 2
                ob, bob = obk.next()
                rb, brb = rbk.next()
                qs = q[:, i * 128:(i + 1) * 128]
                chunks = list(range(i + 1))
                first = True
                for c0 in range(0, len(chunks), 4):
                    grp = chunks[c0:c0 + 4]
                    sb_, bsb = sbk.next()
                    pt, bpt = ptr.next()
                    for jj, j in enumerate(grp):
                        past = (j // 2) < own
                        c.op("pe", lambda: nc.tensor.matmul(sb_[:, jj, :], k[:, j * 128:(j + 1) * 128], qs, start=True, stop=not past),
                             reads=[bk_, bq], writes=[bsb])
                        if past:
                            n = j // 2
                            c.op("pe", lambda: nc.tensor.matmul(sb_[:, jj, :], K.E[:, n * 128:(n + 1) * 128], penT[:, i * 128:(i + 1) * 128],
                                                                start=False, stop=True), reads=[bpenT], writes=[bsb])
                    for jj, j in enumerate(grp):
                        col = h * NQ + (i - j)
                        c.op("act", lambda: nc.scalar.activation(pt[:, jj, :], sb_[:, jj, :], AF.Exp, bias=K.alibi[:, col:col + 1], scale=scale),
                             reads=[bsb], writes=[bpt])
                        if j == i:
                            c.op("pool", lambda: nc.gpsimd.tensor_tensor(pt[:, jj, :], pt[:, jj, :], K.tri[:], ALU.mult),
                                 reads=[bpt], writes=[bpt])
                    for jj, j in enumerate(grp):
                        last = (j == i)
                        c.op("pe", lambda: nc.tensor.matmul(ob[:, 0:128], vt[:, j, :], pt[:, jj, :], start=first, stop=last),
                             reads=[bv, bpt], writes=[bob])
                        c.op("pe", lambda: nc.tensor.matmul(rb[:, 0:128], K.ones_b[:], pt[:, jj, :], start=first, stop=last),
                             reads=[bpt], writes=[brb])
                        first = False
                ri, bri = rir.next()
                c.op("dve", lambda: nc.vector.reciprocal(ri[:], rb[:, 0:128]), reads=[brb], writes=[bri])
                c.op("dve", lambda: nc.vector.tensor_tensor(os_[:, i * 128:(i + 1) * 128], ob[:, 0:128], ri[:], ALU.mult),
                     reads=[bob, bri], writes=[bos])
            c.dma(attnT[h * 128:(h + 1) * 128, :], os_[:], reads=[bos], sem_buf=bos)


def merge_phase(c, ya, yb, yc, wA, wB, wC, sg, mT, T):
    nc = c.nc
    TS = 512
    srcs = [y.rearrange("(k p) t -> p k t", p=128) for y in (ya, yb, yc)]
    ws = (wA, wB, wC)
    sgv = sg.rearrange("(b d) t -> d b t", b=3)
    with c.phase():
        act = Ring(c, "mg_a", [128, 3, 8, TS], BF16, 2)
        wt = Ring(c, "mg_w", [128, 3, 8, 256], BF16, 2)
        gr = Ring(c, "mg_g", [128, 3, 512], BF16, 3)
        tr = Ring(c, "mg_t", [128, 3, 512], F32, 2)
        mr = Ring(c, "mg_m", [128, 512], BF16, 3)
        banks = Ring(c, "mg_bk", [128, 512], F32, 6, psum=True)
        nsup = T // TS
        seq = [(su, s) for su in range(nsup) for s in range(8)]
        acts, wts = {}, {}

        def load_act(su):
            a, ba = act.next()
            for b in range(3):
                c.dma(a[:, b, :, :], srcs[b][:, :, su * TS:(su + 1) * TS], writes=[ba], sem_buf=ba)
            acts[su] = (a, ba)

        def load_w(i):
            w, bw = wt.next()
            for b in range(3):
                c.dma(w[:, b, :, :], ws[b][seq[i][1]], writes=[bw], sem_buf=bw)
            wts[i] = (w, bw)

        load_act(0)
        load_w(0)
        for i, (su, s) in enumerate(seq):
            if s == 0 and su + 1 < nsup:
                load_act(su + 1)
            if i + 1 < len(seq):
                load_w(i + 1)
            w, bw = wts.pop(i)
            a, ba = acts[su]
            if c.bg is not None:
                c.bg.step(c.bg_rate)
            t0 = su * TS
            for j in range(2):
                d0 = s * 256 + j * 128
                g, bg = gr.next()
                c.dma(g[:], sgv[d0:d0 + 128, :, t0:t0 + 512], writes=[bg], sem_buf=bg)
                bl = [banks.next() for _ in range(3)]
                for b in range(3):
                    for k in range(8):
                        c.op("pe", lambda: nc.tensor.matmul(bl[b][0][:], w[:, b, k, j * 128:(j + 1) * 128], a[:, b, k, :],
                                                            start=(k == 0), stop=(k == 7)), reads=[ba, bw], writes=[bl[b][1]])
                t, bt = tr.next()
                m, bm = mr.next()
                for b in range(3):
                    c.op("dve", lambda: nc.vector.tensor_tensor(t[:, b, :], bl[b][0][:], g[:, b, :], ALU.mult),
                         reads=[bl[b][1], bg], writes=[bt])
                c.op("pool", lambda: nc.gpsimd.tensor_tensor(t[:, 0, :], t[:, 0, :], t[:, 1, :], ALU.add), reads=[bt], writes=[bt])
                c.op("pool", lambda: nc.gpsimd.tensor_tensor(m[:], t[:, 0, :], t[:, 2, :], ALU.add), reads=[bt], writes=[bm])
                c.dma(mT[d0:d0 + 128, t0:t0 + 512], m[:], reads=[bm], sem_buf=bm)


def setup_consts(c, nc, K, T, din):
    NQ = T // 128
    gains_d = din("gains", [128, (3 * NL + 1) * 16])
    convw_d = din("convw", [128, NL * 8 * 3])
    pscale_d = din("pscale", [128, NL * 8])
    invd_d = din("invd", [128, 4 * 512])
    negmask_d = din("negmask", [128, NQ * 16])
    alibi_d = din("alibi", [128, NH * NQ])
    tri_d = din("tri", [128, 128])
    ident_d = din("ident", [128, 128])
    E_d = din("Emat", [128, 16 * 128])
    specs = [("gains", gains_d, [128, (3 * NL + 1) * 16], F32), ("convw", convw_d, [128, NL * 8 * 3], F32),
             ("pscale", pscale_d, [128, NL * 8], F32),
             ("invd", invd_d.rearrange("p (g t) -> p g t", g=4), [128, 4, 512], F32),
             ("negmask", negmask_d, [128, NQ * 16], F32), ("alibi", alibi_d, [128, NH * NQ], F32),
             ("tri", tri_d, [128, 128], BF16), ("ident", ident_d, [128, 128], BF16),
             ("E", E_d, [128, 16 * 128], BF16)]
    for (name, src, shape, dt) in specs:
        setattr(K, name, c.sbuf("k_" + name, shape, dt, persistent=True))
    K.ones_f = c.sbuf("k_ones_f", [128, 128], F32, persistent=True)
    K.ones_b = c.sbuf("k_ones_b", [128, 128], BF16, persistent=True)
    with c.phase():
        for (name, src, shape, dt) in specs:
            t = getattr(K, name)
            b = Buf("k_" + name)
            if dt == F32:
                c.dma(t[:], src, writes=[b], sem_buf=b)
            else:
                st = c.sbuf("ks_" + name, shape, F32)
                bs = Buf("ks_" + name)
                c.dma(st[:], src, writes=[bs], sem_buf=bs)
                c.op("dve", lambda: nc.vector.tensor_copy(t[:], st[:]), reads=[bs], writes=[b])
        c.op("dve", lambda: nc.vector.memset(K.ones_f[:], 1.0))
        c.op("dve", lambda: nc.vector.memset(K.ones_b[:], 1.0))


class Consts:
    pass


def build(T, stop_after=None, dbg=(), skip_ffn1=False):
    NQ = T // 128
    nc = bass.Bass("TRN2", target_bir_lowering=False)

    def din(name, shape):
        return nc.dram_tensor(name, list(shape), F32, kind="ExternalInput").ap()

    def scr(name, shape, dt):
        kind = "ExternalOutput" if name in dbg else "Internal"
        return nc.dram_tensor(name, list(shape), dt, kind=kind).ap()

    xT = din("xT", [D, T])
    W = {}
    W["ffn1_w_in"] = din("ffn1_w_in", [NL, D, 2 * DFF])
    W["ffn1_w_out"] = din("ffn1_w_out", [NL, DFF, D])
    W["mix_w_in"] = din("mix_w_in", [NL, D, INDIM])
    W["conv_w_out"] = din("conv_w_out", [NL, 1024, D])
    W["pool_w"] = din("pool_w", [NL, 4, 256, 256])
    W["pool_w_out"] = din("pool_w_out", [NL, 1024, D])
    W["attn_w_out"] = din("attn_w_out", [NL, 1024, D])
    W["mix_w_out"] = din("mix_w_out", [NL, D, D])
    W["ffn2_w_in"] = din("ffn2_w_in", [NL, D, 2 * DFF])
    W["ffn2_w_out"] = din("ffn2_w_out", [NL, DFF, D])
    yT = nc.dram_tensor("yT", [D, T], F32, kind="ExternalOutput").ap()

    S = {}
    for l in range(NL):
        S["f1i", l] = scr("s_f1i%d" % l, [22, 128, 16, 512], BF16)
        S["f1o", l] = scr("s_f1o%d" % l, [8, 128, 44, 256], BF16)
        S["mi", l] = scr("s_mi%d" % l, [26, 128, 16, 512], BF16)
        S["cwo", l] = scr("s_cwo%d" % l, [8, 128, 8, 256], BF16)
        S["pwo", l] = scr("s_pwo%d" % l, [8, 128, 8, 256], BF16)
        S["awo", l] = scr("s_awo%d" % l, [8, 128, 8, 256], BF16)
        S["mo", l] = scr("s_mo%d" % l, [8, 128, 16, 256], BF16)
        S["f2i", l] = scr("s_f2i%d" % l, [22, 128, 16, 512], BF16)
        S["f2o", l] = scr("s_f2o%d" % l, [8, 128, 44, 256], BF16)
    xres = scr("xres", [D, T], F32)
    hT = scr("hT", [D, T], BF16)
    gT = scr("gT", [DFF, T], BF16)
    pa = scr("pa", [4096, T], F32)
    qk = scr("qk", [2048, T], BF16)
    vS = scr("vS", [T, 1024], BF16)
    kbar = scr("kbar", [128, NH * 16], BF16)
    sgS = scr("sgS", [3 * D, T], BF16)
    ya = scr("ya", [1024, T], BF16)
    yb = scr("yb", [1024, T], BF16)
    yc = scr("yc", [1024, T], BF16)
    mT = scr("mT", [D, T], BF16)

    c = Ctx(nc)
    K = Consts()
    bg = BgCast(c)

    class Stop(Exception):
        pass

    def check(name):
        if stop_after == name:
            raise Stop()

    ffn_in_runs = [(0, 22, 256, 0, 0), (DFF, 22, 256, 0, 256)]
    std8 = [(0, 8, 256, 0, 0)]
    for l in range(NL):
        if not skip_ffn1:
            bg.add(("f1i", l), W["ffn1_w_in"][l], D, ffn_in_runs, S["f1i", l])
            bg.add(("f1o", l), W["ffn1_w_out"][l], DFF, std8, S["f1o", l])
        bg.add(("mi", l), W["mix_w_in"][l], D, [(0, 26, 512, 0, 0)], S["mi", l])
        bg.add(("cwo", l), W["conv_w_out"][l], 1024, std8, S["cwo", l])
        bg.add(("pwo", l), W["pool_w_out"][l], 1024, std8, S["pwo", l])
        bg.add(("awo", l), W["attn_w_out"][l], 1024, std8, S["awo", l])
        bg.add(("mo", l), W["mix_w_out"][l], D, std8, S["mo", l])
        bg.add(("f2i", l), W["ffn2_w_in"][l], D, ffn_in_runs, S["f2i", l])
        bg.add(("f2o", l), W["ffn2_w_out"][l], DFF, std8, S["f2o", l])
    c.bg = bg
    c.bg_rate = 2

    def need(*keys):
        todo = [k for k in keys if bg.left.get(k, 0) > 0]
        if todo:
            with c.phase():
                for k in todo:
                    bg.ensure(k)
                while bg.loaded or bg.casted:
                    if bg.casted:
                        bg._store()
                    if bg.loaded:
                        bg._cast()

    try:
        setup_consts(c, nc, K, T, din)
        check("consts")
        xcur = xT
        for l in range(NL):
            if not skip_ffn1:
                need(("f1i", l))
                norm_phase(c, K, xcur, hT, BF16, K.gains[:, (l * 3 + 0) * 16:(l * 3 + 1) * 16], T)
                check("norm1")
                linear_phase(c, hT, D, S["f1i", l], 22, 512, 1024, T, EpiFfnIn(gT))
                check("ffn1_in")
                need(("f1o", l))
                linear_phase(c, gT, DFF, S["f1o", l], 8, 256, 1024, T, EpiResid(xcur, xres, 0.5), nact=1)
                xcur = xres
                check("ffn1")
            norm_phase(c, K, xcur, hT, BF16, K.gains[:, (l * 3 + 1) * 16:(l * 3 + 2) * 16], T)
            need(("mi", l))
            linear_phase(c, hT, D, S["mi", l], 26, 512, 1024, T, EpiMixIn(pa, qk, vS, kbar, sgS, T), tokmajor=(12, 13))
            check("mix_in")
            conv_phase(c, K, l, pa, ya, T)
            check("conv")
            pool_phase(c, K, l, pa, W["pool_w"][l], yb, T)
            check("pool")
            attn_phase(c, K, qk, vS, kbar, yc, T)
            check("attn")
            need(("cwo", l), ("pwo", l), ("awo", l))
            merge_phase(c, ya, yb, yc, S["cwo", l], S["pwo", l], S["awo", l], sgS, mT, T)
            check("merge")
            need(("mo", l))
            linear_phase(c, mT, D, S["mo", l], 8, 256, 1024, T, EpiResid(xcur, xres, 1.0))
            xcur = xres
            check("mix")
            norm_phase(c, K, xres, hT, BF16, K.gains[:, (l * 3 + 2) * 16:(l * 3 + 3) * 16], T)
            need(("f2i", l))
            linear_phase(c, hT, D, S["f2i", l], 22, 512, 1024, T, EpiFfnIn(gT))
            need(("f2o", l))
            linear_phase(c, gT, DFF, S["f2o", l], 8, 256, 1024, T, EpiResid(xres, xres, 0.5), nact=1)
            check("layer%d" % l)
        norm_phase(c, K, xres, yT, F32, K.gains[:, 3 * NL * 16:(3 * NL + 1) * 16], T)
    except Stop:
        pass
    c.barrier()
    c.es.close()
    return nc, c


def host_consts(T):
    NQ = T // 128
    k = {}
    t = np.arange(512, dtype=np.float32)
    invd = np.stack([1.0 / np.minimum(t + 1.0, float(w)) for w in (2, 4, 8, 16)], 0)
    k["invd"] = np.ascontiguousarray(np.broadcast_to(invd.reshape(1, 4 * 512), (128, 4 * 512))).astype(np.float32)
    nm = np.zeros((NQ, 16), np.float32)
    for i in range(NQ):
        nm[i, (i // 2):] = NEG
    k["negmask"] = np.ascontiguousarray(np.broadcast_to(nm.reshape(1, NQ * 16), (128, NQ * 16))).astype(np.float32)
    slopes = np.exp2(-(8.0 / NH) * np.arange(1, NH + 1, dtype=np.float64))
    ki = np.arange(128, dtype=np.float64)[:, None, None]
    rel = np.arange(NQ, dtype=np.float64)[None, None, :]
    al = slopes[None, :, None] * (ki - rel * 128.0 - 64.0)
    k["alibi"] = np.ascontiguousarray(al.reshape(128, NH * NQ)).astype(np.float32)
    kk = np.arange(128)
    k["tri"] = (kk[:, None] <= kk[None, :]).astype(np.float32)
    k["ident"] = np.eye(128, dtype=np.float32)
    E = np.zeros((128, 16, 128), np.float32)
    for n in range(16):
        E[n, n, :] = 1.0
    k["Emat"] = np.ascontiguousarray(E.reshape(128, 16 * 128))
    return k


def chunked(vec):
    return np.ascontiguousarray(np.asarray(vec, np.float32).reshape(-1, 128).T)


def host_small(inp):
    g = []
    for l in range(NL):
        g += [chunked(inp["ffn1_norm"][l]), chunked(inp["mix_norm"][l]), chunked(inp["ffn2_norm"][l])]
    g.append(chunked(inp["final_norm"]))
    out = {"gains": np.ascontiguousarray(np.concatenate(g, axis=1))}
    cw = np.asarray(inp["conv_w"], np.float32)
    cw = cw.reshape(NL, 3, 8, 128).transpose(3, 0, 2, 1)
    out["convw"] = np.ascontiguousarray(cw.reshape(128, NL * 8 * 3))
    ps = np.asarray(inp["pool_scale"], np.float32).reshape(NL, 8, 128).transpose(2, 0, 1)
    out["pscale"] = np.ascontiguousarray(ps.reshape(128, NL * 8))
    return out


WNAMES = ["ffn1_w_in", "ffn1_w_out", "mix_w_in", "conv_w_out", "pool_w", "pool_w_out", "attn_w_out",
          "mix_w_out", "ffn2_w_in", "ffn2_w_out"]


def kernel(**inputs):
    x = np.asarray(inputs["x"], np.float32)
    B, T, _ = x.shape
    nc, _ = build(T)
    base = {n: np.ascontiguousarray(np.asarray(inputs[n], np.float32)) for n in WNAMES}
    base.update(host_consts(T))
    base.update(host_small(inputs))
    in_maps = []
    for b in range(B):
        m = dict(base)
        m["xT"] = np.ascontiguousarray(x[b].T)
        in_maps.append(m)
    res = run_bass_kernel_spmd(nc, in_maps, core_ids=list(range(B)))
    out = np.stack([np.ascontiguousarray(res.results[b]["yT"].T) for b in range(B)], 0)
    return out.astype(np.float32)
```

```python
from contextlib import ExitStack, contextmanager
import numpy as np
import concourse.bass as bass
import concourse.mybir as mybir
from concourse.bass_utils import run_bass_kernel_spmd

F32 = mybir.dt.float32
BF16 = mybir.dt.bfloat16
AF = mybir.ActivationFunctionType
ALU = mybir.AluOpType
AX = mybir.AxisListType

D = 2048
DFF = 5632
NL = 2
INDIM = 13312
NH = 8
EPS = 1e-6
NEG = -1.0e30
PEN = -30000.0
DBG = {}


class Buf:
    __slots__ = ("name", "w", "r", "dsem", "persist")

    def __init__(self, name, persist=False):
        self.name = name
        self.w = None
        self.r = {}
        self.dsem = None
        self.persist = persist


class SemSlot:
    __slots__ = ("key", "cnt")

    def __init__(self, key):
        self.key = key
        self.cnt = 0


class Ctx:
    NDSEM = 56

    def __init__(self, nc):
        self.nc = nc
        self.es = ExitStack()
        self.pes = None
        self.eng = {"pe": nc.tensor, "act": nc.scalar, "dve": nc.vector,
                    "pool": nc.gpsimd, "sp": nc.sync}
        self.semh = {}
        self.cnt = {}
        for k in self.eng:
            self.semh[k] = self.es.enter_context(nc.semaphore("s_" + k))
            self.cnt[k] = 0
        self.known = {k: {} for k in self.eng}
        self.slots = []
        for i in range(self.NDSEM):
            key = "d%d" % i
            self.semh[key] = self.es.enter_context(nc.semaphore(key))
            self.slots.append(SemSlot(key))
        self.free_slots = list(self.slots)
        self.phase_slots = []
        self.nwaits = 0
        self.ninstr = 0
        self.uid = 0
        self.bg = None

    def sbuf(self, name, shape, dt, persistent=False):
        self.uid += 1
        st = self.es if (persistent or self.pes is None) else self.pes
        return st.enter_context(self.nc.sbuf_tensor("%s_%d" % (name, self.uid), list(shape), dt))

    def psum(self, name, shape, dt):
        self.uid += 1
        return self.pes.enter_context(self.nc.psum_tensor("%s_%d" % (name, self.uid), list(shape), dt))

    def _wait(self, E, deps):
        eng = self.eng[E]
        kn = self.known[E]
        best = {}
        for d in deps:
            if d is None:
                continue
            k, v = d
            if E == "pe" and k == "pe":
                continue
            if best.get(k, 0) < v:
                best[k] = v
        for k, v in best.items():
            if kn.get(k, 0) < v:
                eng.wait_ge(self.semh[k], v)
                kn[k] = v
                self.nwaits += 1

    @staticmethod
    def _deps(reads, writes):
        deps = []
        for b in reads:
            deps.append(b.w)
        for b in writes:
            deps.append(b.w)
            deps.extend(b.r.items())
        return deps

    def op(self, E, fn, reads=(), writes=()):
        self._wait(E, self._deps(reads, writes))
        ins = fn()
        self.cnt[E] += 1
        cn = self.cnt[E]
        ins.then_inc(self.semh[E], 1)
        self.ninstr += 1
        for b in reads:
            if b.r.get(E, 0) < cn:
                b.r[E] = cn
        for b in writes:
            b.w = (E, cn)
            b.r = {}
        return ins

    def dma(self, out_ap, in_ap, reads=(), writes=(), sem_buf=None, Q="sp", **kw):
        self._wait(Q, self._deps(reads, writes))
        sb = sem_buf
        if sb.dsem is None:
            sb.dsem = self.free_slots.pop()
            if not sb.persist:
                self.phase_slots.append(sb.dsem)
        ins = self.eng[Q].dma_start(out=out_ap, in_=in_ap, **kw)
        sb.dsem.cnt += 16
        ins.then_inc(self.semh[sb.dsem.key], 16)
        self.ninstr += 1
        ev = (sb.dsem.key, sb.dsem.cnt)
        for b in reads:
            if b.r.get(ev[0], 0) < ev[1]:
                b.r[ev[0]] = ev[1]
        for b in writes:
            b.w = ev
            b.r = {}
        return ins

    def barrier(self):
        allev = [(k, v) for k, v in self.cnt.items() if v > 0]
        allev += [(s.key, s.cnt) for s in self.slots if s.cnt > 0]
        for E in self.eng:
            self._wait(E, allev)

    @contextmanager
    def phase(self):
        self.pes = ExitStack()
        try:
            yield
            self.barrier()
        finally:
            self.pes.close()
            self.pes = None
            self.free_slots.extend(self.phase_slots)
            self.phase_slots = []


class Ring:
    def __init__(self, c, name, shape, dt, n, psum=False):
        self.t = []
        self.b = []
        for i in range(n):
            nm = "%s%d" % (name, i)
            self.t.append(c.psum(nm, shape, dt) if psum else c.sbuf(nm, shape, dt))
            self.b.append(Buf(nm))
        self.i = 0
        self.n = n

    def next(self):
        i = self.i % self.n
        self.i += 1
        return self.t[i], self.b[i]


class BgCast:
    MAXC = 1664

    def __init__(self, c):
        self.c = c
        n = 3
        self.tin = [c.sbuf("bg_in%d" % i, [128, self.MAXC], F32, persistent=True) for i in range(n)]
        self.tout = [c.sbuf("bg_out%d" % i, [128, self.MAXC], BF16, persistent=True) for i in range(n)]
        self.bin = [Buf("bg_in%d" % i, persist=True) for i in range(n)]
        self.bout = [Buf("bg_out%d" % i, persist=True) for i in range(n)]
        self.n = n
        self.items = []
        self.pos = 0
        self.left = {}
        self.loaded = []
        self.casted = []
        self.k = 0

    def add(self, key, src, K, runs, dst):
        KC = K // 128
        self.left.setdefault(key, 0)
        for kc in range(KC):
            for (col0, nseg, w, s0, c0) in runs:
                per = max(1, self.MAXC // w)
                sg = 0
                while sg < nseg:
                    n = min(per, nseg - sg)
                    a = col0 + sg * w
                    sv = src[kc * 128:(kc + 1) * 128, a:a + n * w]
                    dv = dst[s0 + sg:s0 + sg + n, :, kc, c0:c0 + w].rearrange("s p c -> p s c")
                    self.items.append((key, sv, dv, n, w))
                    self.left[key] += 1
                    sg += n

    def _store(self):
        c = self.c
        key, dv, n, w, i = self.casted.pop(0)
        c.dma(dv, self.tout[i][:, 0:n * w].rearrange("p (s c) -> p s c", c=w), reads=[self.bout[i]], sem_buf=self.bout[i])
        self.left[key] -= 1

    def _cast(self):
        c = self.c
        nc = c.nc
        key, dv, n, w, i = self.loaded.pop(0)
        c.op("pool", lambda: nc.gpsimd.tensor_copy(self.tout[i][:, 0:n * w], self.tin[i][:, 0:n * w]),
             reads=[self.bin[i]], writes=[self.bout[i]])
        self.casted.append((key, dv, n, w, i))

    def _load(self):
        c = self.c
        key, sv, dv, n, w = self.items[self.pos]
        self.pos += 1
        i = self.k % self.n
        self.k += 1
        c.dma(self.tin[i][:, 0:n * w], sv, writes=[self.bin[i]], sem_buf=self.bin[i])
        self.loaded.append((key, dv, n, w, i))

    def step(self, n=1):
        for _ in range(n):
            if self.casted:
                self._store()
            if self.loaded:
                self._cast()
            if self.pos < len(self.items):
                self._load()

    def pending(self):
        return self.pos < len(self.items) or self.loaded or self.casted

    def ensure(self, key):
        while self.left.get(key, 0) > 0:
            self.step()


def cast_weight(c, src, K, runs, dst, eng_rot):
    nc = c.nc
    KC = K // 128
    with c.phase():
        maxc = 3328
        fin = Ring(c, "cw_in", [128, maxc], F32, 3)
        fout = Ring(c, "cw_out", [128, maxc], BF16, 3)
        for kc in range(KC):
            for (col0, nseg, w, s0, c0) in runs:
                per = max(1, maxc // w)
                sg = 0
                while sg < nseg:
                    n = min(per, nseg - sg)
                    ncols = n * w
                    a = col0 + sg * w
                    ti, bi = fin.next()
                    to, bo = fout.next()
                    c.dma(ti[:, 0:ncols], src[kc * 128:(kc + 1) * 128, a:a + ncols], writes=[bi], sem_buf=bi)
                    E = eng_rot[0]
                    eng_rot.append(eng_rot.pop(0))
                    if E == "act":
                        c.op("act", lambda: nc.scalar.copy(to[:, 0:ncols], ti[:, 0:ncols]), reads=[bi], writes=[bo])
                    elif E == "dve":
                        c.op("dve", lambda: nc.vector.tensor_copy(to[:, 0:ncols], ti[:, 0:ncols]), reads=[bi], writes=[bo])
                    else:
                        c.op("pool", lambda: nc.gpsimd.tensor_copy(to[:, 0:ncols], ti[:, 0:ncols]), reads=[bi], writes=[bo])
                    dv = dst[s0 + sg:s0 + sg + n, :, kc, c0:c0 + w].rearrange("s p c -> p s c")
                    c.dma(dv, to[:, 0:ncols].rearrange("p (s c) -> p s c", c=w), reads=[bo], sem_buf=bo)
                    sg += n


def norm_phase(c, K, src, dst, dst_dt, gain_ap, T):
    nc = c.nc
    TT = 512 if dst_dt == BF16 else 256
    srcv = src.rearrange("(k p) t -> p k t", p=128)
    dstv = dst.rearrange("(k p) t -> p k t", p=128)
    with c.phase():
        xr = Ring(c, "nx", [128, 16, TT], F32, 2)
        sr = Ring(c, "nsq", [128, 16, TT], F32, 1 if TT == 512 else 2)
        hr = Ring(c, "nh", [128, 16, TT], dst_dt, 2)
        rr = Ring(c, "nr", [128, TT], F32, 2)
        pr = Ring(c, "nps", [128, 512], F32, 2, psum=True)
        for tt in range(T // TT):
            x, bx = xr.next()
            sq, bs = sr.next()
            h, bh = hr.next()
            r, br = rr.next()
            ps, bp = pr.next()
            c.dma(x[:], srcv[:, :, tt * TT:(tt + 1) * TT], writes=[bx], sem_buf=bx)
            c.op("act", lambda: nc.scalar.activation(sq[:], x[:], AF.Square), reads=[bx], writes=[bs])
            for k in range(16):
                c.op("pe", lambda: nc.tensor.matmul(ps[:, 0:TT], K.ones_f[:], sq[:, k, :], start=(k == 0), stop=(k == 15)),
                     reads=[bs], writes=[bp])
            c.op("dve", lambda: nc.vector.tensor_scalar(r[:], ps[:, 0:TT], 1.0 / D, EPS, ALU.mult, ALU.add),
                 reads=[bp], writes=[br])
            c.op("act", lambda: nc.scalar.activation(r[:], r[:], AF.Sqrt), reads=[br], writes=[br])
            c.op("dve", lambda: nc.vector.reciprocal(r[:], r[:]), reads=[br], writes=[br])
            for k in range(16):
                c.op("dve", lambda: nc.vector.scalar_tensor_tensor(h[:, k, :], x[:, k, :], gain_ap[:, k:k + 1], r[:], ALU.mult, ALU.mult),
                     reads=[bx, br], writes=[bh])
            c.dma(dstv[:, :, tt * TT:(tt + 1) * TT], h[:], reads=[bh], sem_buf=bh)


def linear_phase(c, srcT, Kdim, wsl, S, C, TS, T, epi, nact=2, tokmajor=()):
    nc = c.nc
    KC = Kdim // 128
    G = C // 128
    nsup = T // TS
    nsub = TS // 512
    srcv = srcT.rearrange("(k p) t -> p k t", p=128)
    with c.phase():
        act = Ring(c, "act", [128, KC, TS], BF16, nact)
        wt = Ring(c, "wt", [128, KC, C], BF16, 2)
        banks = Ring(c, "bk", [128, 512], F32, 8, psum=True)
        epi.setup(c)
        seq = [(su, s) for su in range(nsup) for s in range(S)]
        acts = {}

        def load_act(su):
            a, ba = act.next()
            for k0 in range(0, KC, 8):
                k1 = min(KC, k0 + 8)
                c.dma(a[:, k0:k1, :], srcv[:, k0:k1, su * TS:(su + 1) * TS], writes=[ba], sem_buf=ba)
            acts[su] = (a, ba)

        wts = {}

        def load_w(i):
            w, bw = wt.next()
            c.dma(w[:], wsl[seq[i][1]], writes=[bw], sem_buf=bw)
            wts[i] = (w, bw)

        load_act(0)
        load_w(0)
        for i, (su, s) in enumerate(seq):
            if s == 0 and su + 1 < nsup and nact > 1:
                load_act(su + 1)
            if su not in acts:
                load_act(su)
            if i + 1 < len(seq):
                load_w(i + 1)
            w, bw = wts.pop(i)
            a, ba = acts[su]
            if c.bg is not None:
                c.bg.step(c.bg_rate)
            for sb in range(nsub):
                t0 = su * TS + sb * 512
                bl = [banks.next() for _ in range(4 if s in tokmajor else G)]
                if s in tokmajor:
                    for g in range(4):
                        for k in range(KC):
                            c.op("pe", lambda: nc.tensor.matmul(bl[g][0][:, 0:C], a[:, k, sb * 512 + g * 128:sb * 512 + (g + 1) * 128],
                                                                w[:, k, :], start=(k == 0), stop=(k == KC - 1)),
                                 reads=[ba, bw], writes=[bl[g][1]])
                else:
                    for g in range(G):
                        for k in range(KC):
                            c.op("pe", lambda: nc.tensor.matmul(bl[g][0][:], w[:, k, g * 128:(g + 1) * 128],
                                                                a[:, k, sb * 512:(sb + 1) * 512], start=(k == 0), stop=(k == KC - 1)),
                                 reads=[ba, bw], writes=[bl[g][1]])
                epi(c, s, t0, [b[0] for b in bl], [b[1] for b in bl])
        epi.finish(c)


class EpiFfnIn:
    def __init__(self, gT):
        self.gT = gT

    def setup(self, c):
        self.sa = Ring(c, "e_sa", [128, 512], F32, 4)
        self.go = Ring(c, "e_go", [128, 2, 512], BF16, 3)

    def __call__(self, c, s, t0, bk, bb):
        nc = c.nc
        go, bgo = self.go.next()
        for j in range(2):
            sa, bsa = self.sa.next()
            c.op("act", lambda: nc.scalar.activation(sa[:], bk[j][:], AF.Silu), reads=[bb[j]], writes=[bsa])
            c.op("dve", lambda: nc.vector.tensor_tensor(go[:, j, :], sa[:], bk[2 + j][:], ALU.mult),
                 reads=[bsa, bb[2 + j]], writes=[bgo])
        dv = self.gT[s * 256:(s + 1) * 256, t0:t0 + 512].rearrange("(j p) t -> p j t", p=128)
        c.dma(dv, go[:], reads=[bgo], sem_buf=bgo)

    def finish(self, c):
        pass


class EpiResid:
    def __init__(self, xsrc, xdst, coef):
        self.xsrc, self.xdst, self.coef = xsrc, xdst, coef

    def setup(self, c):
        self.xr = Ring(c, "e_x", [128, 2, 512], F32, 4)

    def __call__(self, c, s, t0, bk, bb):
        nc = c.nc
        x, bx = self.xr.next()
        sv = self.xsrc[s * 256:(s + 1) * 256, t0:t0 + 512].rearrange("(j p) t -> p j t", p=128)
        dv = self.xdst[s * 256:(s + 1) * 256, t0:t0 + 512].rearrange("(j p) t -> p j t", p=128)
        c.dma(x[:], sv, writes=[bx], sem_buf=bx)
        for j in range(2):
            c.op("dve", lambda: nc.vector.scalar_tensor_tensor(x[:, j, :], bk[j][:], float(self.coef), x[:, j, :], ALU.mult, ALU.add),
                 reads=[bb[j], bx], writes=[bx])
        c.dma(dv, x[:], reads=[bx], sem_buf=bx)

    def finish(self, c):
        pass


class EpiMixIn:
    def __init__(self, pa, qk, v, kbar, sg, T):
        self.pa, self.qk, self.v, self.kbar, self.sg, self.T = pa, qk, v, kbar, sg, T

    def setup(self, c):
        self.f = Ring(c, "e_f", [128, 4, 512], F32, 2)
        self.h = Ring(c, "e_h", [128, 4, 512], BF16, 3)
        self.kf = Ring(c, "e_kf", [128, 512], F32, 2)
        self.kb = c.sbuf("e_kb", [128, 8, self.T // 256], F32)
        self.bkb = Buf("e_kb")
        self.kbo = c.sbuf("e_kbo", [128, 8, 16], BF16)
        self.bkbo = Buf("e_kbo")
        self.n = 0

    def __call__(self, c, s, t0, bk, bb):
        nc = c.nc
        self.n += 1
        if s < 8:
            f, bf = self.f.next()
            for j in range(4):
                if (self.n + j) % 2 == 0:
                    c.op("act", lambda: nc.scalar.copy(f[:, j, :], bk[j][:]), reads=[bb[j]], writes=[bf])
                else:
                    c.op("dve", lambda: nc.vector.tensor_copy(f[:, j, :], bk[j][:]), reads=[bb[j]], writes=[bf])
            dv = self.pa[s * 512:(s + 1) * 512, t0:t0 + 512].rearrange("(j p) t -> p j t", p=128)
            c.dma(dv, f[:], reads=[bf], sem_buf=bf)
        elif s < 12:
            h, bh = self.h.next()
            for j in range(4):
                if s < 10:
                    if (self.n + j) % 2 == 0:
                        c.op("act", lambda: nc.scalar.copy(h[:, j, :], bk[j][:]), reads=[bb[j]], writes=[bh])
                    else:
                        c.op("dve", lambda: nc.vector.tensor_copy(h[:, j, :], bk[j][:]), reads=[bb[j]], writes=[bh])
                else:
                    hd = (s - 10) * 4 + j
                    b0 = t0 // 256
                    kf, bkf = self.kf.next()
                    c.op("act", lambda: nc.scalar.copy(kf[:], bk[j][:]), reads=[bb[j]], writes=[bkf])
                    c.op("pool", lambda: nc.gpsimd.tensor_copy(h[:, j, :], kf[:]), reads=[bkf], writes=[bh])
                    for hb in range(2):
                        c.op("dve", lambda: nc.vector.reduce_sum(self.kb[:, hd, b0 + hb:b0 + hb + 1], kf[:, hb * 256:(hb + 1) * 256], AX.X),
                             reads=[bkf], writes=[self.bkb])
            r0 = (s - 8) * 512
            dv = self.qk[r0:r0 + 512, t0:t0 + 512].rearrange("(j p) t -> p j t", p=128)
            c.dma(dv, h[:], reads=[bh], sem_buf=bh)
        elif s < 14:
            h, bh = self.h.next()
            for g in range(4):
                if (self.n + g) % 2 == 0:
                    c.op("act", lambda: nc.scalar.copy(h[:, g, :], bk[g][:]), reads=[bb[g]], writes=[bh])
                else:
                    c.op("dve", lambda: nc.vector.tensor_copy(h[:, g, :], bk[g][:]), reads=[bb[g]], writes=[bh])
            c0 = (s - 12) * 512
            dv = self.v[t0:t0 + 512, c0:c0 + 512].rearrange("(g p) f -> p g f", p=128)
            c.dma(dv, h[:], reads=[bh], sem_buf=bh)
        else:
            h, bh = self.h.next()
            for j in range(4):
                c.op("act", lambda: nc.scalar.activation(h[:, j, :], bk[j][:], AF.Sigmoid), reads=[bb[j]], writes=[bh])
            r0 = (s - 14) * 512
            dv = self.sg[r0:r0 + 512, t0:t0 + 512].rearrange("(j p) t -> p j t", p=128)
            c.dma(dv, h[:], reads=[bh], sem_buf=bh)

    def finish(self, c):
        nc = c.nc
        NB = self.T // 256
        c.op("dve", lambda: nc.vector.memset(self.kbo[:], 0.0), writes=[self.bkbo])
        c.op("dve", lambda: nc.vector.tensor_scalar(self.kbo[:, :, 0:NB], self.kb[:], 1.0 / 256, None, ALU.mult),
             reads=[self.bkb], writes=[self.bkbo])
        c.dma(self.kbar.rearrange("p (h n) -> p h n", n=16), self.kbo[:], reads=[self.bkbo], sem_buf=self.bkbo)


def conv_phase(c, K, l, pa, ya, T):
    nc = c.nc
    TT = 1024
    cxv = pa[1024:3072, :].rearrange("(two c) t -> c two t", two=2)
    with c.phase():
        cxr = Ring(c, "cv_cx", [128, 2, TT + 2], F32, 2)
        btr = Ring(c, "cv_b", [128, TT], F32, 2)
        ur = Ring(c, "cv_u", [128, TT + 2], F32, 2)
        ar = Ring(c, "cv_a", [128, TT], F32, 2)
        yr = Ring(c, "cv_y", [128, TT], BF16, 2)
        for ch in range(8):
            wb = (l * 8 + ch) * 3
            for tt in range(T // TT):
                t0 = tt * TT
                cx, bcx = cxr.next()
                bt, bbt = btr.next()
                u, bu = ur.next()
                a, ba = ar.next()
                y, by = yr.next()
                if t0 == 0:
                    c.op("pool", lambda: nc.gpsimd.memset(cx[:, :, 0:2], 0.0), writes=[bcx])
                    c.dma(cx[:, :, 2:], cxv[ch * 128:(ch + 1) * 128, :, 0:TT], writes=[bcx], sem_buf=bcx)
                else:
                    c.dma(cx[:], cxv[ch * 128:(ch + 1) * 128, :, t0 - 2:t0 + TT], writes=[bcx], sem_buf=bcx)
                c.dma(bt[:], pa[ch * 128:(ch + 1) * 128, t0:t0 + TT], writes=[bbt], sem_buf=bbt)
                c.op("dve", lambda: nc.vector.tensor_tensor(u[:], cx[:, 0, :], cx[:, 1, :], ALU.mult), reads=[bcx], writes=[bu])
                c.op("dve", lambda: nc.vector.tensor_scalar(a[:], u[:, 2:TT + 2], K.convw[:, wb + 2:wb + 3], None, ALU.mult),
                     reads=[bu], writes=[ba])
                c.op("dve", lambda: nc.vector.scalar_tensor_tensor(a[:], u[:, 1:TT + 1], K.convw[:, wb + 1:wb + 2], a[:], ALU.mult, ALU.add),
                     reads=[bu, ba], writes=[ba])
                c.op("dve", lambda: nc.vector.scalar_tensor_tensor(a[:], u[:, 0:TT], K.convw[:, wb:wb + 1], a[:], ALU.mult, ALU.add),
                     reads=[bu, ba], writes=[ba])
                c.op("dve", lambda: nc.vector.tensor_tensor(y[:], bt[:], a[:], ALU.mult), reads=[bbt, ba], writes=[by])
                c.dma(ya[ch * 128:(ch + 1) * 128, t0:t0 + TT], y[:], reads=[by], sem_buf=by)


def pool_phase(c, K, l, pa, pool_w_l, yb, T):
    nc = c.nc
    TT = 512
    H = 16
    with c.phase():
        pwf = c.sbuf("pl_wf", [128, 4, 2, 256], F32)
        pwb = c.sbuf("pl_wb", [128, 4, 2, 256], BF16)
        bpwf, bpwb = Buf("pl_wf"), Buf("pl_wb")
        c.dma(pwf[:], pool_w_l.rearrange("g (k p) d -> p g k d", p=128), writes=[bpwf], sem_buf=bpwf)
        c.op("dve", lambda: nc.vector.tensor_copy(pwb[:], pwf[:]), reads=[bpwf], writes=[bpwb])
        pr = Ring(c, "pl_p", [128, 2, TT + H], F32, 2)
        s1r = Ring(c, "pl_s1", [128, 2, TT + H], F32, 2)
        s2r = Ring(c, "pl_s2", [128, 2, TT + H], F32, 2)
        dr = Ring(c, "pl_d", [128, 2, TT], BF16, 2)
        yr = Ring(c, "pl_y", [128, 2, TT], BF16, 2)
        banks = Ring(c, "pl_bk", [128, 512], F32, 4, psum=True)
        for g in range(4):
            w = 2 << g
            pv = pa[3072 + g * 256:3072 + (g + 1) * 256, :].rearrange("(ci p) t -> p ci t", p=128)
            for tt in range(T // TT):
                t0 = tt * TT
                p, bp = pr.next()
                s1, bs1 = s1r.next()
                s2, bs2 = s2r.next()
                d, bd = dr.next()
                y, by = yr.next()
                if t0 == 0:
                    c.op("pool", lambda: nc.gpsimd.memset(p[:, :, 0:H], 0.0), writes=[bp])
                    c.dma(p[:, :, H:], pv[:, :, 0:TT], writes=[bp], sem_buf=bp)
                else:
                    c.dma(p[:], pv[:, :, t0 - H:t0 + TT], writes=[bp], sem_buf=bp)
                cur, bcur = p, bp
                sh = 1
                tog = 0
                while sh < w:
                    nxt, bnxt = (s1, bs1) if tog == 0 else (s2, bs2)
                    tog ^= 1
                    E = "dve"
                    eng = nc.vector
                    c.op(E, lambda: eng.tensor_tensor(nxt[:, :, sh:], cur[:, :, sh:], cur[:, :, 0:TT + H - sh], ALU.add),
                         reads=[bcur], writes=[bnxt])
                    cur, bcur = nxt, bnxt
                    sh *= 2
                if t0 == 0:
                    for ci in range(2):
                        c.op("dve", lambda: nc.vector.tensor_tensor(cur[:, ci, H:], cur[:, ci, H:], K.invd[:, g, :], ALU.mult),
                             reads=[bcur], writes=[bcur])
                    c.op("dve", lambda: nc.vector.tensor_tensor(d[:], cur[:, :, H:], p[:, :, H:], ALU.subtract),
                         reads=[bcur, bp], writes=[bd])
                else:
                    c.op("dve", lambda: nc.vector.scalar_tensor_tensor(d[:], cur[:, :, H:], 1.0 / w, p[:, :, H:], ALU.mult, ALU.subtract),
                         reads=[bcur, bp], writes=[bd])
                for j in range(2):
                    bk, bbk = banks.next()
                    for ci in range(2):
                        c.op("pe", lambda: nc.tensor.matmul(bk[:], pwb[:, g, ci, j * 128:(j + 1) * 128], d[:, ci, :],
                                                            start=(ci == 0), stop=(ci == 1)),
                             reads=[bpwb, bd], writes=[bbk])
                    sc = l * 8 + g * 2 + j
                    c.op("act", lambda: nc.scalar.activation(y[:, j, :], bk[:], AF.Copy, scale=K.pscale[:, sc:sc + 1]),
                         reads=[bbk], writes=[by])
                dv = yb[g * 256:(g + 1) * 256, t0:t0 + TT].rearrange("(j p) t -> p j t", p=128)
                c.dma(dv, y[:], reads=[by], sem_buf=by)


def attn_phase(c, K, qk, v, kbar, attnT, T):
    nc = c.nc
    NQ = T // 128
    scale = 128.0 ** -0.5
    vv = v.rearrange("(c p) f -> p c f", p=128)
    with c.phase():
        qr = Ring(c, "at_q", [128, T], BF16, 2)
        kr = Ring(c, "at_k", [128, T], BF16, 2)
        vr = Ring(c, "at_v", [128, NQ, 128], BF16, 2)
        kbr = Ring(c, "at_kb", [128, 16], BF16, 2)
        gm = c.sbuf("at_gm", [128, NQ, 16], F32)
        bgm = Buf("at_gm")
        top = c.sbuf("at_top", [128, NQ, 8], F32)
        btop = Buf("at_top")
        thr = c.sbuf("at_thr", [128, NQ], F32)
        bthr = Buf("at_thr")
        pen = c.sbuf("at_pen", [128, NQ, 16], BF16)
        bpen = Buf("at_pen")
        penT = c.sbuf("at_penT", [128, T], BF16)
        bpenT = Buf("at_penT")
        c.op("pool", lambda: nc.gpsimd.memset(penT[:], 0.0), writes=[bpenT])
        ptr = Ring(c, "at_pt", [128, 4, 128], BF16, 3)
        rir = Ring(c, "at_ri", [128, 128], F32, 2)
        osr = Ring(c, "at_os", [128, T], BF16, 2)
        gbank = c.psum("at_gb", [128, 512], F32)
        bgbank = Buf("at_gb")
        tbank = c.psum("at_tb", [16, 1024], BF16)
        btbank = Buf("at_tb")
        sbk = Ring(c, "at_s", [128, 4, 128], F32, 2, psum=True)
        obk = Ring(c, "at_o", [128, 512], F32, 2, psum=True)
        rbk = Ring(c, "at_r", [128, 512], F32, 2, psum=True)

        heads = {}

        def load_head(h):
            q, bq = qr.next()
            k, bk_ = kr.next()
            vt, bv = vr.next()
            kb, bkb = kbr.next()
            c.dma(q[:], qk[h * 128:(h + 1) * 128, :], writes=[bq], sem_buf=bq)
            c.dma(k[:], qk[1024 + h * 128:1024 + (h + 1) * 128, :], writes=[bk_], sem_buf=bk_)
            for c0 in range(0, NQ, 8):
                c.dma(vt[:, c0:c0 + 8, :], vv[:, c0:c0 + 8, h * 128:(h + 1) * 128], writes=[bv], sem_buf=bv)
            c.dma(kb[:], kbar[:, h * 16:(h + 1) * 16], writes=[bkb], sem_buf=bkb)
            heads[h] = (q, bq, k, bk_, vt, bv, kb, bkb)

        load_head(0)
        for h in range(NH):
            if h + 1 < NH:
                load_head(h + 1)
            q, bq, k, bk_, vt, bv, kb, bkb = heads.pop(h)
            for i in range(NQ):
                c.op("pe", lambda: nc.tensor.matmul(gbank[:, i * 16:(i + 1) * 16], q[:, i * 128:(i + 1) * 128], kb[:],
                                                    start=True, stop=True), reads=[bq, bkb], writes=[bgbank])
            c.op("dve", lambda: nc.vector.tensor_tensor(gm[:].rearrange("p a b -> p (a b)"), gbank[:, 0:NQ * 16], K.negmask[:], ALU.add),
                 reads=[bgbank], writes=[bgm])
            for i in range(NQ):
                c.op("dve", lambda: nc.vector.max(out=top[:, i, :], in_=gm[:, i, :]), reads=[bgm], writes=[btop])
            c.op("dve", lambda: nc.vector.tensor_scalar(thr[:], top[:, :, 2], -1.0e29, None, ALU.max), reads=[btop], writes=[bthr])
            for i in range(NQ):
                c.op("dve", lambda: nc.vector.tensor_scalar(pen[:, i, :], gm[:, i, :], thr[:, i:i + 1], PEN, ALU.is_lt, ALU.mult),
                     reads=[bgm, bthr], writes=[bpen])
            for i0 in range(0, NQ, 8):
                for i in range(i0, min(i0 + 8, NQ)):
                    c.op("pe", lambda: nc.tensor.transpose(tbank[:, (i - i0) * 128:(i - i0 + 1) * 128], pen[:, i, :], K.ident[:]),
                         reads=[bpen], writes=[btbank])
                nn = min(8, NQ - i0) * 128
                c.op("act", lambda: nc.scalar.copy(penT[0:16, i0 * 128:i0 * 128 + nn], tbank[:, 0:nn]), reads=[btbank], writes=[bpenT])
            os_, bos = osr.next()
            G = []
            for i in range(NQ):
                chunks = list(range(i + 1))
                for c0 in range(0, len(chunks), 4):
                    G.append({"i": i, "grp": chunks[c0:c0 + 4], "first": c0 == 0, "last": c0 + 4 >= len(chunks)})
            acc = {}

            def emit_qk(g):
                i = g["i"]
                own = i // 2
                sb_, bsb = sbk.next()
                g["s"] = (sb_, bsb)
                qs = q[:, i * 128:(i + 1) * 128]
                for jj, j in enumerate(g["grp"]):
                    past = (j // 2) < own
                    c.op("pe", lambda: nc.tensor.matmul(sb_[:, jj, :], k[:, j * 128:(j + 1) * 128], qs, start=True, stop=not past),
                         reads=[bk_, bq], writes=[bsb])
                    if past:
                        n = j // 2
                        c.op("pe", lambda: nc.tensor.matmul(sb_[:, jj, :], K.E[:, n * 128:(n + 1) * 128], penT[:, i * 128:(i + 1) * 128],
                                                            start=False, stop=True), reads=[bpenT], writes=[bsb])

            def emit_exp(g):
                i = g["i"]
                sb_, bsb = g["s"]
                pt, bpt = ptr.next()
                g["p"] = (pt, bpt)
                for jj, j in enumerate(g["grp"]):
                    col = h * NQ + (i - j)
                    c.op("act", lambda: nc.scalar.activation(pt[:, jj, :], sb_[:, jj, :], AF.Exp, bias=K.alibi[:, col:col + 1], scale=scale),
                         reads=[bsb], writes=[bpt])
                    if j == i:
                        c.op("pool", lambda: nc.gpsimd.tensor_tensor(pt[:, jj, :], pt[:, jj, :], K.tri[:], ALU.mult),
                             reads=[bpt], writes=[bpt])

            def emit_pv(g):
                i = g["i"]
                pt, bpt = g["p"]
                if g["first"]:
                    acc[i] = (obk.next(), rbk.next())
                (ob, bob), (rb, brb) = acc[i]
                for jj, j in enumerate(g["grp"]):
                    st = g["first"] and jj == 0
                    last = (j == i)
                    c.op("pe", lambda: nc.tensor.matmul(ob[:, 0:128], vt[:, j, :], pt[:, jj, :], start=st, stop=last),
                         reads=[bv, bpt], writes=[bob])
                    c.op("pe", lambda: nc.tensor.matmul(rb[:, 0:128], K.ones_b[:], pt[:, jj, :], start=st, stop=last),
                         reads=[bpt], writes=[brb])
                if g["last"]:
                    ri, bri = rir.next()
                    c.op("dve", lambda: nc.vector.reciprocal(ri[:], rb[:, 0:128]), reads=[brb], writes=[bri])
                    c.op("dve", lambda: nc.vector.tensor_tensor(os_[:, i * 128:(i + 1) * 128], ob[:, 0:128], ri[:], ALU.mult),
                         reads=[bob, bri], writes=[bos])
                    del acc[i]

            emit_qk(G[0])
            for n in range(len(G)):
                if n + 1 < len(G):
                    emit_qk(G[n + 1])
                emit_exp(G[n])
                emit_pv(G[n])
            c.dma(attnT[h * 128:(h + 1) * 128, :], os_[:], reads=[bos], sem_buf=bos)


def merge_phase(c, ya, yb, yc, wA, wB, wC, sg, mT, T):
    nc = c.nc
    TS = 1024
    srcs = [y.rearrange("(k p) t -> p k t", p=128) for y in (ya, yb, yc)]
    ws = (wA, wB, wC)
    sgv = sg.rearrange("(b d) t -> d b t", b=3)
    with c.phase():
        act = Ring(c, "mg_a", [128, 3, 8, TS], BF16, 1)
        wt = Ring(c, "mg_w", [128, 3, 8, 256], BF16, 3)
        gr = Ring(c, "mg_g", [128, 3, 512], BF16, 3)
        tr = Ring(c, "mg_t", [128, 3, 512], F32, 2)
        mr = Ring(c, "mg_m", [128, 512], BF16, 3)
        banks = Ring(c, "mg_bk", [128, 512], F32, 6, psum=True)
        nsup = T // TS
        seq = [(su, s) for su in range(nsup) for s in range(8)]
        acts, wts = {}, {}

        def load_act(su):
            a, ba = act.next()
            for b in range(3):
                c.dma(a[:, b, :, :], srcs[b][:, :, su * TS:(su + 1) * TS], writes=[ba], sem_buf=ba)
            acts[su] = (a, ba)

        def load_w(i):
            w, bw = wt.next()
            for b in range(3):
                c.dma(w[:, b, :, :], ws[b][seq[i][1]], writes=[bw], sem_buf=bw)
            wts[i] = (w, bw)

        load_w(0)
        load_w(1)
        for i, (su, s) in enumerate(seq):
            if su not in acts:
                load_act(su)
            if i + 2 < len(seq):
                load_w(i + 2)
            w, bw = wts.pop(i)
            a, ba = acts[su]
            if c.bg is not None:
                c.bg.step(c.bg_rate)
            for sb in range(TS // 512):
                t0 = su * TS + sb * 512
                for j in range(2):
                    d0 = s * 256 + j * 128
                    g, bg = gr.next()
                    c.dma(g[:], sgv[d0:d0 + 128, :, t0:t0 + 512], writes=[bg], sem_buf=bg)
                    bl = [banks.next() for _ in range(3)]
                    for b in range(3):
                        for k in range(8):
                            c.op("pe", lambda: nc.tensor.matmul(bl[b][0][:], w[:, b, k, j * 128:(j + 1) * 128],
                                                                a[:, b, k, sb * 512:(sb + 1) * 512],
                                                                start=(k == 0), stop=(k == 7)), reads=[ba, bw], writes=[bl[b][1]])
                    t, bt = tr.next()
                    m, bm = mr.next()
                    for b in range(3):
                        c.op("dve", lambda: nc.vector.tensor_tensor(t[:, b, :], bl[b][0][:], g[:, b, :], ALU.mult),
                             reads=[bl[b][1], bg], writes=[bt])
                    c.op("dve", lambda: nc.vector.tensor_tensor(t[:, 0, :], t[:, 0, :], t[:, 1, :], ALU.add), reads=[bt], writes=[bt])
                    c.op("dve", lambda: nc.vector.tensor_tensor(m[:], t[:, 0, :], t[:, 2, :], ALU.add), reads=[bt], writes=[bm])
                    c.dma(mT[d0:d0 + 128, t0:t0 + 512], m[:], reads=[bm], sem_buf=bm)


def setup_consts(c, nc, K, T, din):
    NQ = T // 128
    gains_d = din("gains", [128, (3 * NL + 1) * 16])
    convw_d = din("convw", [128, NL * 8 * 3])
    pscale_d = din("pscale", [128, NL * 8])
    invd_d = din("invd", [128, 4 * 512])
    negmask_d = din("negmask", [128, NQ * 16])
    alibi_d = din("alibi", [128, NH * NQ])
    tri_d = din("tri", [128, 128])
    ident_d = din("ident", [128, 128])
    E_d = din("Emat", [128, 16 * 128])
    specs = [("gains", gains_d, [128, (3 * NL + 1) * 16], F32), ("convw", convw_d, [128, NL * 8 * 3], F32),
             ("pscale", pscale_d, [128, NL * 8], F32),
             ("invd", invd_d.rearrange("p (g t) -> p g t", g=4), [128, 4, 512], F32),
             ("negmask", negmask_d, [128, NQ * 16], F32), ("alibi", alibi_d, [128, NH * NQ], F32),
             ("tri", tri_d, [128, 128], BF16), ("ident", ident_d, [128, 128], BF16),
             ("E", E_d, [128, 16 * 128], BF16)]
    for (name, src, shape, dt) in specs:
        setattr(K, name, c.sbuf("k_" + name, shape, dt, persistent=True))
    K.ones_f = c.sbuf("k_ones_f", [128, 128], F32, persistent=True)
    K.ones_b = c.sbuf("k_ones_b", [128, 128], BF16, persistent=True)
    with c.phase():
        for (name, src, shape, dt) in specs:
            t = getattr(K, name)
            b = Buf("k_" + name)
            if dt == F32:
                c.dma(t[:], src, writes=[b], sem_buf=b)
            else:
                st = c.sbuf("ks_" + name, shape, F32)
                bs = Buf("ks_" + name)
                c.dma(st[:], src, writes=[bs], sem_buf=bs)
                c.op("dve", lambda: nc.vector.tensor_copy(t[:], st[:]), reads=[bs], writes=[b])
        c.op("dve", lambda: nc.vector.memset(K.ones_f[:], 1.0))
        c.op("dve", lambda: nc.vector.memset(K.ones_b[:], 1.0))


class Consts:
    pass


def build(T, stop_after=None, dbg=(), skip_ffn1=False):
    NQ = T // 128
    nc = bass.Bass("TRN2", target_bir_lowering=False)

    def din(name, shape):
        return nc.dram_tensor(name, list(shape), F32, kind="ExternalInput").ap()

    def scr(name, shape, dt):
        kind = "ExternalOutput" if name in dbg else "Internal"
        return nc.dram_tensor(name, list(shape), dt, kind=kind).ap()

    xT = din("xT", [D, T])
    W = {}
    W["ffn1_w_in"] = din("ffn1_w_in", [NL, D, 2 * DFF])
    W["ffn1_w_out"] = din("ffn1_w_out", [NL, DFF, D])
    W["mix_w_in"] = din("mix_w_in", [NL, D, INDIM])
    W["conv_w_out"] = din("conv_w_out", [NL, 1024, D])
    W["pool_w"] = din("pool_w", [NL, 4, 256, 256])
    W["pool_w_out"] = din("pool_w_out", [NL, 1024, D])
    W["attn_w_out"] = din("attn_w_out", [NL, 1024, D])
    W["mix_w_out"] = din("mix_w_out", [NL, D, D])
    W["ffn2_w_in"] = din("ffn2_w_in", [NL, D, 2 * DFF])
    W["ffn2_w_out"] = din("ffn2_w_out", [NL, DFF, D])
    yT = nc.dram_tensor("yT", [D, T], F32, kind="ExternalOutput").ap()

    S = {}
    for l in range(NL):
        S["f1i", l] = scr("s_f1i%d" % l, [22, 128, 16, 512], BF16)
        S["f1o", l] = scr("s_f1o%d" % l, [8, 128, 44, 256], BF16)
        S["mi", l] = scr("s_mi%d" % l, [26, 128, 16, 512], BF16)
        S["cwo", l] = scr("s_cwo%d" % l, [8, 128, 8, 256], BF16)
        S["pwo", l] = scr("s_pwo%d" % l, [8, 128, 8, 256], BF16)
        S["awo", l] = scr("s_awo%d" % l, [8, 128, 8, 256], BF16)
        S["mo", l] = scr("s_mo%d" % l, [8, 128, 16, 256], BF16)
        S["f2i", l] = scr("s_f2i%d" % l, [22, 128, 16, 512], BF16)
        S["f2o", l] = scr("s_f2o%d" % l, [8, 128, 44, 256], BF16)
    xres = scr("xres", [D, T], F32)
    hT = scr("hT", [D, T], BF16)
    gT = scr("gT", [DFF, T], BF16)
    pa = scr("pa", [4096, T], F32)
    qk = scr("qk", [2048, T], BF16)
    vS = scr("vS", [T, 1024], BF16)
    kbar = scr("kbar", [128, NH * 16], BF16)
    sgS = scr("sgS", [3 * D, T], BF16)
    ya = scr("ya", [1024, T], BF16)
    yb = scr("yb", [1024, T], BF16)
    yc = scr("yc", [1024, T], BF16)
    mT = scr("mT", [D, T], BF16)

    c = Ctx(nc)
    K = Consts()
    bg = BgCast(c)

    class Stop(Exception):
        pass

    def check(name):
        if stop_after == name:
            raise Stop()

    ffn_in_runs = [(0, 22, 256, 0, 0), (DFF, 22, 256, 0, 256)]
    std8 = [(0, 8, 256, 0, 0)]
    for l in range(NL):
        if not skip_ffn1:
            bg.add(("f1i", l), W["ffn1_w_in"][l], D, ffn_in_runs, S["f1i", l])
            bg.add(("f1o", l), W["ffn1_w_out"][l], DFF, std8, S["f1o", l])
        bg.add(("mi", l), W["mix_w_in"][l], D, [(0, 26, 512, 0, 0)], S["mi", l])
        bg.add(("cwo", l), W["conv_w_out"][l], 1024, std8, S["cwo", l])
        bg.add(("pwo", l), W["pool_w_out"][l], 1024, std8, S["pwo", l])
        bg.add(("awo", l), W["attn_w_out"][l], 1024, std8, S["awo", l])
        bg.add(("mo", l), W["mix_w_out"][l], D, std8, S["mo", l])
        bg.add(("f2i", l), W["ffn2_w_in"][l], D, ffn_in_runs, S["f2i", l])
        bg.add(("f2o", l), W["ffn2_w_out"][l], DFF, std8, S["f2o", l])
    c.bg = bg
    c.bg_rate = 2

    def need(*keys):
        todo = [k for k in keys if bg.left.get(k, 0) > 0]
        if todo:
            with c.phase():
                for k in todo:
                    bg.ensure(k)
                while bg.loaded or bg.casted:
                    if bg.casted:
                        bg._store()
                    if bg.loaded:
                        bg._cast()

    try:
        setup_consts(c, nc, K, T, din)
        check("consts")
        xcur = xT
        for l in range(NL):
            if not skip_ffn1:
                need(("f1i", l))
                norm_phase(c, K, xcur, hT, BF16, K.gains[:, (l * 3 + 0) * 16:(l * 3 + 1) * 16], T)
                check("norm1")
                linear_phase(c, hT, D, S["f1i", l], 22, 512, 1024, T, EpiFfnIn(gT))
                check("ffn1_in")
                need(("f1o", l))
                linear_phase(c, gT, DFF, S["f1o", l], 8, 256, 1024, T, EpiResid(xcur, xres, 0.5), nact=1)
                xcur = xres
                check("ffn1")
            norm_phase(c, K, xcur, hT, BF16, K.gains[:, (l * 3 + 1) * 16:(l * 3 + 2) * 16], T)
            need(("mi", l))
            linear_phase(c, hT, D, S["mi", l], 26, 512, 1024, T, EpiMixIn(pa, qk, vS, kbar, sgS, T), tokmajor=(12, 13))
            check("mix_in")
            conv_phase(c, K, l, pa, ya, T)
            check("conv")
            pool_phase(c, K, l, pa, W["pool_w"][l], yb, T)
            check("pool")
            attn_phase(c, K, qk, vS, kbar, yc, T)
            check("attn")
            need(("cwo", l), ("pwo", l), ("awo", l))
            merge_phase(c, ya, yb, yc, S["cwo", l], S["pwo", l], S["awo", l], sgS, mT, T)
            check("merge")
            need(("mo", l))
            linear_phase(c, mT, D, S["mo", l], 8, 256, 1024, T, EpiResid(xcur, xres, 1.0))
            xcur = xres
            check("mix")
            norm_phase(c, K, xres, hT, BF16, K.gains[:, (l * 3 + 2) * 16:(l * 3 + 3) * 16], T)
            need(("f2i", l))
            linear_phase(c, hT, D, S["f2i", l], 22, 512, 1024, T, EpiFfnIn(gT))
            need(("f2o", l))
            linear_phase(c, gT, DFF, S["f2o", l], 8, 256, 1024, T, EpiResid(xres, xres, 0.5), nact=1)
            check("layer%d" % l)
        norm_phase(c, K, xres, yT, F32, K.gains[:, 3 * NL * 16:(3 * NL + 1) * 16], T)
    except Stop:
        pass
    c.barrier()
    c.es.close()
    return nc, c


def host_consts(T):
    NQ = T // 128
    k = {}
    t = np.arange(512, dtype=np.float32)
    invd = np.stack([1.0 / np.minimum(t + 1.0, float(w)) for w in (2, 4, 8, 16)], 0)
    k["invd"] = np.ascontiguousarray(np.broadcast_to(invd.reshape(1, 4 * 512), (128, 4 * 512))).astype(np.float32)
    nm = np.zeros((NQ, 16), np.float32)
    for i in range(NQ):
        nm[i, (i // 2):] = NEG
    k["negmask"] = np.ascontiguousarray(np.broadcast_to(nm.reshape(1, NQ * 16), (128, NQ * 16))).astype(np.float32)
    slopes = np.exp2(-(8.0 / NH) * np.arange(1, NH + 1, dtype=np.float64))
    ki = np.arange(128, dtype=np.float64)[:, None, None]
    rel = np.arange(NQ, dtype=np.float64)[None, None, :]
    al = slopes[None, :, None] * (ki - rel * 128.0 - 64.0)
    k["alibi"] = np.ascontiguousarray(al.reshape(128, NH * NQ)).astype(np.float32)
    kk = np.arange(128)
    k["tri"] = (kk[:, None] <= kk[None, :]).astype(np.float32)
    k["ident"] = np.eye(128, dtype=np.float32)
    E = np.zeros((128, 16, 128), np.float32)
    for n in range(16):
        E[n, n, :] = 1.0
    k["Emat"] = np.ascontiguousarray(E.reshape(128, 16 * 128))
    return k


def chunked(vec):
    return np.ascontiguousarray(np.asarray(vec, np.float32).reshape(-1, 128).T)


def host_small(inp):
    g = []
    for l in range(NL):
        g += [chunked(inp["ffn1_norm"][l]), chunked(inp["mix_norm"][l]), chunked(inp["ffn2_norm"][l])]
    g.append(chunked(inp["final_norm"]))
    out = {"gains": np.ascontiguousarray(np.concatenate(g, axis=1))}
    cw = np.asarray(inp["conv_w"], np.float32)
    cw = cw.reshape(NL, 3, 8, 128).transpose(3, 0, 2, 1)
    out["convw"] = np.ascontiguousarray(cw.reshape(128, NL * 8 * 3))
    ps = np.asarray(inp["pool_scale"], np.float32).reshape(NL, 8, 128).transpose(2, 0, 1)
    out["pscale"] = np.ascontiguousarray(ps.reshape(128, NL * 8))
    return out


WNAMES = ["ffn1_w_in", "ffn1_w_out", "mix_w_in", "conv_w_out", "pool_w", "pool_w_out", "attn_w_out",
          "mix_w_out", "ffn2_w_in", "ffn2_w_out"]


def kernel(**inputs):
    x = np.asarray(inputs["x"], np.float32)
    B, T, _ = x.shape
    nc, _ = build(T)
    base = {n: np.ascontiguousarray(np.asarray(inputs[n], np.float32)) for n in WNAMES}
    base.update(host_consts(T))
    base.update(host_small(inputs))
    in_maps = []
    for b in range(B):
        m = dict(base)
        m["xT"] = np.ascontiguousarray(x[b].T)
        in_maps.append(m)
    res = run_bass_kernel_spmd(nc, in_maps, core_ids=list(range(B)))
    out = np.stack([np.ascontiguousarray(res.results[b]["yT"].T) for b in range(B)], 0)
    return out.astype(np.float32)
```

```python
from contextlib import ExitStack, contextmanager
import numpy as np
import concourse.bass as bass
import concourse.mybir as mybir
from concourse.bass_utils import run_bass_kernel_spmd

F32 = mybir.dt.float32
BF16 = mybir.dt.bfloat16
AF = mybir.ActivationFunctionType
ALU = mybir.AluOpType
AX = mybir.AxisListType

D = 2048
DFF = 5632
NL = 2
INDIM = 13312
NH = 8
EPS = 1e-6
NEG = -1.0e30
PEN = -30000.0
DBG = {}


class Buf:
    __slots__ = ("name", "w", "r", "dsem", "persist")

    def __init__(self, name, persist=False):
        self.name = name
        self.w = None
        self.r = {}
        self.dsem = None
        self.persist = persist


class SemSlot:
    __slots__ = ("key", "cnt")

    def __init__(self, key):
        self.key = key
        self.cnt = 0


class Ctx:
    NDSEM = 56

    def __init__(self, nc):
        self.nc = nc
        self.es = ExitStack()
        self.pes = None
        self.eng = {"pe": nc.tensor, "act": nc.scalar, "dve": nc.vector,
                    "pool": nc.gpsimd, "sp": nc.sync}
        self.semh = {}
        self.cnt = {}
        for k in self.eng:
            self.semh[k] = self.es.enter_context(nc.semaphore("s_" + k))
            self.cnt[k] = 0
        self.known = {k: {} for k in self.eng}
        self.slots = []
        for i in range(self.NDSEM):
            key = "d%d" % i
            self.semh[key] = self.es.enter_context(nc.semaphore(key))
            self.slots.append(SemSlot(key))
        self.free_slots = list(self.slots)
        self.phase_slots = []
        self.nwaits = 0
        self.ninstr = 0
        self.uid = 0
        self.bg = None

    def sbuf(self, name, shape, dt, persistent=False):
        self.uid += 1
        st = self.es if (persistent or self.pes is None) else self.pes
        return st.enter_context(self.nc.sbuf_tensor("%s_%d" % (name, self.uid), list(shape), dt))

    def psum(self, name, shape, dt):
        self.uid += 1
        return self.pes.enter_context(self.nc.psum_tensor("%s_%d" % (name, self.uid), list(shape), dt))

    def _wait(self, E, deps):
        eng = self.eng[E]
        kn = self.known[E]
        best = {}
        for d in deps:
            if d is None:
                continue
            k, v = d
            if E == "pe" and k == "pe":
                continue
            if best.get(k, 0) < v:
                best[k] = v
        for k, v in best.items():
            if kn.get(k, 0) < v:
                eng.wait_ge(self.semh[k], v)
                kn[k] = v
                self.nwaits += 1

    @staticmethod
    def _deps(reads, writes):
        deps = []
        for b in reads:
            deps.append(b.w)
        for b in writes:
            deps.append(b.w)
            deps.extend(b.r.items())
        return deps

    def op(self, E, fn, reads=(), writes=()):
        self._wait(E, self._deps(reads, writes))
        ins = fn()
        self.cnt[E] += 1
        cn = self.cnt[E]
        ins.then_inc(self.semh[E], 1)
        self.ninstr += 1
        for b in reads:
            if b.r.get(E, 0) < cn:
                b.r[E] = cn
        for b in writes:
            b.w = (E, cn)
            b.r = {}
        return ins

    def dma(self, out_ap, in_ap, reads=(), writes=(), sem_buf=None, Q="sp", **kw):
        self._wait(Q, self._deps(reads, writes))
        sb = sem_buf
        if sb.dsem is None:
            sb.dsem = self.free_slots.pop()
            if not sb.persist:
                self.phase_slots.append(sb.dsem)
        ins = self.eng[Q].dma_start(out=out_ap, in_=in_ap, **kw)
        sb.dsem.cnt += 16
        ins.then_inc(self.semh[sb.dsem.key], 16)
        self.ninstr += 1
        ev = (sb.dsem.key, sb.dsem.cnt)
        for b in reads:
            if b.r.get(ev[0], 0) < ev[1]:
                b.r[ev[0]] = ev[1]
        for b in writes:
            b.w = ev
            b.r = {}
        return ins

    def barrier(self):
        allev = [(k, v) for k, v in self.cnt.items() if v > 0]
        allev += [(s.key, s.cnt) for s in self.slots if s.cnt > 0]
        for E in self.eng:
            self._wait(E, allev)

    @contextmanager
    def phase(self):
        self.pes = ExitStack()
        try:
            yield
            self.barrier()
        finally:
            self.pes.close()
            self.pes = None
            self.free_slots.extend(self.phase_slots)
            self.phase_slots = []


class Ring:
    def __init__(self, c, name, shape, dt, n, psum=False):
        self.t = []
        self.b = []
        for i in range(n):
            nm = "%s%d" % (name, i)
            self.t.append(c.psum(nm, shape, dt) if psum else c.sbuf(nm, shape, dt))
            self.b.append(Buf(nm))
        self.i = 0
        self.n = n

    def next(self):
        i = self.i % self.n
        self.i += 1
        return self.t[i], self.b[i]


class BgCast:
    MAXC = 1664

    def __init__(self, c):
        self.c = c
        n = 3
        self.tin = [c.sbuf("bg_in%d" % i, [128, self.MAXC], F32, persistent=True) for i in range(n)]
        self.tout = [c.sbuf("bg_out%d" % i, [128, self.MAXC], BF16, persistent=True) for i in range(n)]
        self.bin = [Buf("bg_in%d" % i, persist=True) for i in range(n)]
        self.bout = [Buf("bg_out%d" % i, persist=True) for i in range(n)]
        self.n = n
        self.items = []
        self.pos = 0
        self.left = {}
        self.loaded = []
        self.casted = []
        self.k = 0

    def add(self, key, src, K, runs, dst):
        KC = K // 128
        self.left.setdefault(key, 0)
        for kc in range(KC):
            for (col0, nseg, w, s0, c0) in runs:
                per = max(1, self.MAXC // w)
                sg = 0
                while sg < nseg:
                    n = min(per, nseg - sg)
                    a = col0 + sg * w
                    sv = src[kc * 128:(kc + 1) * 128, a:a + n * w]
                    dv = dst[s0 + sg:s0 + sg + n, :, kc, c0:c0 + w].rearrange("s p c -> p s c")
                    self.items.append((key, sv, dv, n, w))
                    self.left[key] += 1
                    sg += n

    def _store(self):
        c = self.c
        key, dv, n, w, i = self.casted.pop(0)
        c.dma(dv, self.tout[i][:, 0:n * w].rearrange("p (s c) -> p s c", c=w), reads=[self.bout[i]], sem_buf=self.bout[i])
        self.left[key] -= 1

    def _cast(self):
        c = self.c
        nc = c.nc
        key, dv, n, w, i = self.loaded.pop(0)
        c.op("pool", lambda: nc.gpsimd.tensor_copy(self.tout[i][:, 0:n * w], self.tin[i][:, 0:n * w]),
             reads=[self.bin[i]], writes=[self.bout[i]])
        self.casted.append((key, dv, n, w, i))

    def _load(self):
        c = self.c
        key, sv, dv, n, w = self.items[self.pos]
        self.pos += 1
        i = self.k % self.n
        self.k += 1
        c.dma(self.tin[i][:, 0:n * w], sv, writes=[self.bin[i]], sem_buf=self.bin[i])
        self.loaded.append((key, dv, n, w, i))

    def step(self, n=1):
        for _ in range(n):
            if self.casted:
                self._store()
            if self.loaded:
                self._cast()
            if self.pos < len(self.items):
                self._load()

    def pending(self):
        return self.pos < len(self.items) or self.loaded or self.casted

    def ensure(self, key):
        while self.left.get(key, 0) > 0:
            self.step()


def cast_weight(c, src, K, runs, dst, eng_rot):
    nc = c.nc
    KC = K // 128
    with c.phase():
        maxc = 3328
        fin = Ring(c, "cw_in", [128, maxc], F32, 3)
        fout = Ring(c, "cw_out", [128, maxc], BF16, 3)
        for kc in range(KC):
            for (col0, nseg, w, s0, c0) in runs:
                per = max(1, maxc // w)
                sg = 0
                while sg < nseg:
                    n = min(per, nseg - sg)
                    ncols = n * w
                    a = col0 + sg * w
                    ti, bi = fin.next()
                    to, bo = fout.next()
                    c.dma(ti[:, 0:ncols], src[kc * 128:(kc + 1) * 128, a:a + ncols], writes=[bi], sem_buf=bi)
                    E = eng_rot[0]
                    eng_rot.append(eng_rot.pop(0))
                    if E == "act":
                        c.op("act", lambda: nc.scalar.copy(to[:, 0:ncols], ti[:, 0:ncols]), reads=[bi], writes=[bo])
                    elif E == "dve":
                        c.op("dve", lambda: nc.vector.tensor_copy(to[:, 0:ncols], ti[:, 0:ncols]), reads=[bi], writes=[bo])
                    else:
                        c.op("pool", lambda: nc.gpsimd.tensor_copy(to[:, 0:ncols], ti[:, 0:ncols]), reads=[bi], writes=[bo])
                    dv = dst[s0 + sg:s0 + sg + n, :, kc, c0:c0 + w].rearrange("s p c -> p s c")
                    c.dma(dv, to[:, 0:ncols].rearrange("p (s c) -> p s c", c=w), reads=[bo], sem_buf=bo)
                    sg += n


def norm_phase(c, K, src, dst, dst_dt, gain_ap, T):
    nc = c.nc
    TT = 512 if dst_dt == BF16 else 256
    srcv = src.rearrange("(k p) t -> p k t", p=128)
    dstv = dst.rearrange("(k p) t -> p k t", p=128)
    with c.phase():
        xr = Ring(c, "nx", [128, 16, TT], F32, 2)
        sr = Ring(c, "nsq", [128, 16, TT], BF16, 2)
        hr = Ring(c, "nh", [128, 16, TT], dst_dt, 2)
        rr = Ring(c, "nr", [128, TT], F32, 2)
        pr = Ring(c, "nps", [128, 512], F32, 2, psum=True)
        def nload(tt):
            x, bx = xr.next()
            c.dma(x[:], srcv[:, :, tt * TT:(tt + 1) * TT], writes=[bx], sem_buf=bx)
            return x, bx

        nxt = nload(0)
        for tt in range(T // TT):
            x, bx = nxt
            if tt + 1 < T // TT:
                nxt = nload(tt + 1)
            sq, bs = sr.next()
            h, bh = hr.next()
            r, br = rr.next()
            ps, bp = pr.next()
            c.op("act", lambda: nc.scalar.activation(sq[:], x[:], AF.Square), reads=[bx], writes=[bs])
            for k in range(16):
                c.op("pe", lambda: nc.tensor.matmul(ps[:, 0:TT], K.ones_b[:], sq[:, k, :], start=(k == 0), stop=(k == 15)),
                     reads=[bs], writes=[bp])
            c.op("dve", lambda: nc.vector.tensor_scalar(r[:], ps[:, 0:TT], 1.0 / D, EPS, ALU.mult, ALU.add),
                 reads=[bp], writes=[br])
            c.op("act", lambda: nc.scalar.activation(r[:], r[:], AF.Sqrt), reads=[br], writes=[br])
            c.op("dve", lambda: nc.vector.reciprocal(r[:], r[:]), reads=[br], writes=[br])
            for k in range(16):
                c.op("dve", lambda: nc.vector.scalar_tensor_tensor(h[:, k, :], x[:, k, :], gain_ap[:, k:k + 1], r[:], ALU.mult, ALU.mult),
                     reads=[bx, br], writes=[bh])
            c.dma(dstv[:, :, tt * TT:(tt + 1) * TT], h[:], reads=[bh], sem_buf=bh)


def linear_phase(c, srcT, Kdim, wsl, S, C, TS, T, epi, nact=2, tokmajor=()):
    nc = c.nc
    KC = Kdim // 128
    G = C // 128
    nsup = T // TS
    nsub = TS // 512
    srcv = srcT.rearrange("(k p) t -> p k t", p=128)
    with c.phase():
        act = Ring(c, "act", [128, KC, TS], BF16, nact)
        wt = Ring(c, "wt", [128, KC, C], BF16, 2)
        banks = Ring(c, "bk", [128, 512], F32, 8, psum=True)
        epi.setup(c)
        seq = [(su, s) for su in range(nsup) for s in range(S)]
        acts = {}

        def load_act(su):
            a, ba = act.next()
            for k0 in range(0, KC, 8):
                k1 = min(KC, k0 + 8)
                c.dma(a[:, k0:k1, :], srcv[:, k0:k1, su * TS:(su + 1) * TS], writes=[ba], sem_buf=ba)
            acts[su] = (a, ba)

        wts = {}

        def load_w(i):
            w, bw = wt.next()
            c.dma(w[:], wsl[seq[i][1]], writes=[bw], sem_buf=bw)
            wts[i] = (w, bw)

        load_act(0)
        load_w(0)
        for i, (su, s) in enumerate(seq):
            if s == 0 and su + 1 < nsup and nact > 1:
                load_act(su + 1)
            if su not in acts:
                load_act(su)
            if i + 1 < len(seq):
                load_w(i + 1)
            w, bw = wts.pop(i)
            a, ba = acts[su]
            if c.bg is not None:
                c.bg.step(c.bg_rate)
            for sb in range(nsub):
                t0 = su * TS + sb * 512
                bl = [banks.next() for _ in range(4 if s in tokmajor else G)]
                if s in tokmajor:
                    for g in range(4):
                        for k in range(KC):
                            c.op("pe", lambda: nc.tensor.matmul(bl[g][0][:, 0:C], a[:, k, sb * 512 + g * 128:sb * 512 + (g + 1) * 128],
                                                                w[:, k, :], start=(k == 0), stop=(k == KC - 1)),
                                 reads=[ba, bw], writes=[bl[g][1]])
                else:
                    for g in range(G):
                        for k in range(KC):
                            c.op("pe", lambda: nc.tensor.matmul(bl[g][0][:], w[:, k, g * 128:(g + 1) * 128],
                                                                a[:, k, sb * 512:(sb + 1) * 512], start=(k == 0), stop=(k == KC - 1)),
                                 reads=[ba, bw], writes=[bl[g][1]])
                epi(c, s, t0, [b[0] for b in bl], [b[1] for b in bl])
        epi.finish(c)


class EpiFfnIn:
    def __init__(self, gT):
        self.gT = gT

    def setup(self, c):
        self.sa = Ring(c, "e_sa", [128, 512], F32, 4)
        self.go = Ring(c, "e_go", [128, 2, 512], BF16, 3)

    def __call__(self, c, s, t0, bk, bb):
        nc = c.nc
        go, bgo = self.go.next()
        for j in range(2):
            sa, bsa = self.sa.next()
            c.op("act", lambda: nc.scalar.activation(sa[:], bk[j][:], AF.Silu), reads=[bb[j]], writes=[bsa])
            c.op("dve", lambda: nc.vector.tensor_tensor(go[:, j, :], sa[:], bk[2 + j][:], ALU.mult),
                 reads=[bsa, bb[2 + j]], writes=[bgo])
        dv = self.gT[s * 256:(s + 1) * 256, t0:t0 + 512].rearrange("(j p) t -> p j t", p=128)
        c.dma(dv, go[:], reads=[bgo], sem_buf=bgo)

    def finish(self, c):
        pass


class EpiResid:
    def __init__(self, xsrc, xdst, coef):
        self.xsrc, self.xdst, self.coef = xsrc, xdst, coef

    def setup(self, c):
        self.xr = Ring(c, "e_x", [128, 2, 512], F32, 4)

    def __call__(self, c, s, t0, bk, bb):
        nc = c.nc
        x, bx = self.xr.next()
        sv = self.xsrc[s * 256:(s + 1) * 256, t0:t0 + 512].rearrange("(j p) t -> p j t", p=128)
        dv = self.xdst[s * 256:(s + 1) * 256, t0:t0 + 512].rearrange("(j p) t -> p j t", p=128)
        c.dma(x[:], sv, writes=[bx], sem_buf=bx)
        for j in range(2):
            c.op("dve", lambda: nc.vector.scalar_tensor_tensor(x[:, j, :], bk[j][:], float(self.coef), x[:, j, :], ALU.mult, ALU.add),
                 reads=[bb[j], bx], writes=[bx])
        c.dma(dv, x[:], reads=[bx], sem_buf=bx)

    def finish(self, c):
        pass


class EpiMixIn:
    def __init__(self, pa, qk, v, kbar, sg, T):
        self.pa, self.qk, self.v, self.kbar, self.sg, self.T = pa, qk, v, kbar, sg, T

    def setup(self, c):
        self.f = Ring(c, "e_f", [128, 4, 512], F32, 2)
        self.h = Ring(c, "e_h", [128, 4, 512], BF16, 3)
        self.kf = Ring(c, "e_kf", [128, 512], F32, 2)
        self.kb = c.sbuf("e_kb", [128, 8, self.T // 256], F32)
        self.bkb = Buf("e_kb")
        self.kbo = c.sbuf("e_kbo", [128, 8, 16], BF16)
        self.bkbo = Buf("e_kbo")
        self.n = 0

    def __call__(self, c, s, t0, bk, bb):
        nc = c.nc
        self.n += 1
        if s < 8:
            f, bf = self.f.next()
            for j in range(4):
                if (self.n + j) % 2 == 0:
                    c.op("act", lambda: nc.scalar.copy(f[:, j, :], bk[j][:]), reads=[bb[j]], writes=[bf])
                else:
                    c.op("dve", lambda: nc.vector.tensor_copy(f[:, j, :], bk[j][:]), reads=[bb[j]], writes=[bf])
            dv = self.pa[s * 512:(s + 1) * 512, t0:t0 + 512].rearrange("(j p) t -> p j t", p=128)
            c.dma(dv, f[:], reads=[bf], sem_buf=bf)
        elif s < 12:
            h, bh = self.h.next()
            for j in range(4):
                if s < 10:
                    if (self.n + j) % 2 == 0:
                        c.op("act", lambda: nc.scalar.copy(h[:, j, :], bk[j][:]), reads=[bb[j]], writes=[bh])
                    else:
                        c.op("dve", lambda: nc.vector.tensor_copy(h[:, j, :], bk[j][:]), reads=[bb[j]], writes=[bh])
                else:
                    hd = (s - 10) * 4 + j
                    b0 = t0 // 256
                    kf, bkf = self.kf.next()
                    c.op("act", lambda: nc.scalar.copy(kf[:], bk[j][:]), reads=[bb[j]], writes=[bkf])
                    c.op("pool", lambda: nc.gpsimd.tensor_copy(h[:, j, :], kf[:]), reads=[bkf], writes=[bh])
                    for hb in range(2):
                        c.op("dve", lambda: nc.vector.reduce_sum(self.kb[:, hd, b0 + hb:b0 + hb + 1], kf[:, hb * 256:(hb + 1) * 256], AX.X),
                             reads=[bkf], writes=[self.bkb])
            r0 = (s - 8) * 512
            dv = self.qk[r0:r0 + 512, t0:t0 + 512].rearrange("(j p) t -> p j t", p=128)
            c.dma(dv, h[:], reads=[bh], sem_buf=bh)
        elif s < 14:
            h, bh = self.h.next()
            for g in range(4):
                if (self.n + g) % 2 == 0:
                    c.op("act", lambda: nc.scalar.copy(h[:, g, :], bk[g][:]), reads=[bb[g]], writes=[bh])
                else:
                    c.op("dve", lambda: nc.vector.tensor_copy(h[:, g, :], bk[g][:]), reads=[bb[g]], writes=[bh])
            c0 = (s - 12) * 512
            dv = self.v[t0:t0 + 512, c0:c0 + 512].rearrange("(g p) f -> p g f", p=128)
            c.dma(dv, h[:], reads=[bh], sem_buf=bh)
        else:
            h, bh = self.h.next()
            for j in range(4):
                c.op("act", lambda: nc.scalar.activation(h[:, j, :], bk[j][:], AF.Sigmoid), reads=[bb[j]], writes=[bh])
            r0 = (s - 14) * 512
            dv = self.sg[r0:r0 + 512, t0:t0 + 512].rearrange("(j p) t -> p j t", p=128)
            c.dma(dv, h[:], reads=[bh], sem_buf=bh)

    def finish(self, c):
        nc = c.nc
        NB = self.T // 256
        c.op("dve", lambda: nc.vector.memset(self.kbo[:], 0.0), writes=[self.bkbo])
        c.op("dve", lambda: nc.vector.tensor_scalar(self.kbo[:, :, 0:NB], self.kb[:], 1.0 / 256, None, ALU.mult),
             reads=[self.bkb], writes=[self.bkbo])
        c.dma(self.kbar.rearrange("p (h n) -> p h n", n=16), self.kbo[:], reads=[self.bkbo], sem_buf=self.bkbo)


def conv_phase(c, K, l, pa, ya, T):
    nc = c.nc
    TT = 1024
    cxv = pa[1024:3072, :].rearrange("(two c) t -> c two t", two=2)
    with c.phase():
        cxr = Ring(c, "cv_cx", [128, 2, TT + 2], F32, 2)
        btr = Ring(c, "cv_b", [128, TT], F32, 2)
        ur = Ring(c, "cv_u", [128, TT + 2], F32, 2)
        ar = Ring(c, "cv_a", [128, TT], F32, 2)
        yr = Ring(c, "cv_y", [128, TT], BF16, 2)
        def cload(ch, tt):
            t0 = tt * TT
            cx, bcx = cxr.next()
            bt, bbt = btr.next()
            if t0 == 0:
                c.op("pool", lambda: nc.gpsimd.memset(cx[:, :, 0:2], 0.0), writes=[bcx])
                c.dma(cx[:, :, 2:], cxv[ch * 128:(ch + 1) * 128, :, 0:TT], writes=[bcx], sem_buf=bcx)
            else:
                c.dma(cx[:], cxv[ch * 128:(ch + 1) * 128, :, t0 - 2:t0 + TT], writes=[bcx], sem_buf=bcx)
            c.dma(bt[:], pa[ch * 128:(ch + 1) * 128, t0:t0 + TT], writes=[bbt], sem_buf=bbt)
            return cx, bcx, bt, bbt

        citems = [(ch, tt) for ch in range(8) for tt in range(T // TT)]
        cnxt = cload(*citems[0])
        for ci, (ch, tt) in enumerate(citems):
            if True:
                wb = (l * 8 + ch) * 3
                t0 = tt * TT
                cx, bcx, bt, bbt = cnxt
                if ci + 1 < len(citems):
                    cnxt = cload(*citems[ci + 1])
                u, bu = ur.next()
                a, ba = ar.next()
                y, by = yr.next()
                c.op("dve", lambda: nc.vector.tensor_tensor(u[:], cx[:, 0, :], cx[:, 1, :], ALU.mult), reads=[bcx], writes=[bu])
                c.op("dve", lambda: nc.vector.tensor_scalar(a[:], u[:, 2:TT + 2], K.convw[:, wb + 2:wb + 3], None, ALU.mult),
                     reads=[bu], writes=[ba])
                c.op("dve", lambda: nc.vector.scalar_tensor_tensor(a[:], u[:, 1:TT + 1], K.convw[:, wb + 1:wb + 2], a[:], ALU.mult, ALU.add),
                     reads=[bu, ba], writes=[ba])
                c.op("dve", lambda: nc.vector.scalar_tensor_tensor(a[:], u[:, 0:TT], K.convw[:, wb:wb + 1], a[:], ALU.mult, ALU.add),
                     reads=[bu, ba], writes=[ba])
                c.op("dve", lambda: nc.vector.tensor_tensor(y[:], bt[:], a[:], ALU.mult), reads=[bbt, ba], writes=[by])
                c.dma(ya[ch * 128:(ch + 1) * 128, t0:t0 + TT], y[:], reads=[by], sem_buf=by)


def pool_phase(c, K, l, pa, pool_w_l, yb, T):
    nc = c.nc
    TT = 512
    H = 16
    with c.phase():
        pwf = c.sbuf("pl_wf", [128, 4, 2, 256], F32)
        pwb = c.sbuf("pl_wb", [128, 4, 2, 256], BF16)
        bpwf, bpwb = Buf("pl_wf"), Buf("pl_wb")
        c.dma(pwf[:], pool_w_l.rearrange("g (k p) d -> p g k d", p=128), writes=[bpwf], sem_buf=bpwf)
        c.op("dve", lambda: nc.vector.tensor_copy(pwb[:], pwf[:]), reads=[bpwf], writes=[bpwb])
        pr = Ring(c, "pl_p", [128, 2, TT + H], F32, 2)
        s1r = Ring(c, "pl_s1", [128, 2, TT + H], F32, 2)
        s2r = Ring(c, "pl_s2", [128, 2, TT + H], F32, 2)
        dr = Ring(c, "pl_d", [128, 2, TT], BF16, 2)
        yr = Ring(c, "pl_y", [128, 2, TT], BF16, 2)
        banks = Ring(c, "pl_bk", [128, 512], F32, 4, psum=True)
        def pload(g, tt):
            t0 = tt * TT
            pv = pa[3072 + g * 256:3072 + (g + 1) * 256, :].rearrange("(ci p) t -> p ci t", p=128)
            p, bp = pr.next()
            if t0 == 0:
                c.op("pool", lambda: nc.gpsimd.memset(p[:, :, 0:H], 0.0), writes=[bp])
                c.dma(p[:, :, H:], pv[:, :, 0:TT], writes=[bp], sem_buf=bp)
            else:
                c.dma(p[:], pv[:, :, t0 - H:t0 + TT], writes=[bp], sem_buf=bp)
            return p, bp

        pitems = [(g, tt) for g in range(4) for tt in range(T // TT)]
        pnxt = pload(*pitems[0])
        for pi, (g, tt) in enumerate(pitems):
            if True:
                w = 2 << g
                t0 = tt * TT
                p, bp = pnxt
                if pi + 1 < len(pitems):
                    pnxt = pload(*pitems[pi + 1])
                s1, bs1 = s1r.next()
                s2, bs2 = s2r.next()
                d, bd = dr.next()
                y, by = yr.next()
                cur, bcur = p, bp
                sh = 1
                tog = 0
                while sh < w:
                    nxt, bnxt = (s1, bs1) if tog == 0 else (s2, bs2)
                    tog ^= 1
                    E = "dve"
                    eng = nc.vector
                    c.op(E, lambda: eng.tensor_tensor(nxt[:, :, sh:], cur[:, :, sh:], cur[:, :, 0:TT + H - sh], ALU.add),
                         reads=[bcur], writes=[bnxt])
                    cur, bcur = nxt, bnxt
                    sh *= 2
                if t0 == 0:
                    for ci in range(2):
                        c.op("dve", lambda: nc.vector.tensor_tensor(cur[:, ci, H:], cur[:, ci, H:], K.invd[:, g, :], ALU.mult),
                             reads=[bcur], writes=[bcur])
                    c.op("dve", lambda: nc.vector.tensor_tensor(d[:], cur[:, :, H:], p[:, :, H:], ALU.subtract),
                         reads=[bcur, bp], writes=[bd])
                else:
                    c.op("dve", lambda: nc.vector.scalar_tensor_tensor(d[:], cur[:, :, H:], 1.0 / w, p[:, :, H:], ALU.mult, ALU.subtract),
                         reads=[bcur, bp], writes=[bd])
                for j in range(2):
                    bk, bbk = banks.next()
                    for ci in range(2):
                        c.op("pe", lambda: nc.tensor.matmul(bk[:], pwb[:, g, ci, j * 128:(j + 1) * 128], d[:, ci, :],
                                                            start=(ci == 0), stop=(ci == 1)),
                             reads=[bpwb, bd], writes=[bbk])
                    sc = l * 8 + g * 2 + j
                    c.op("act", lambda: nc.scalar.activation(y[:, j, :], bk[:], AF.Copy, scale=K.pscale[:, sc:sc + 1]),
                         reads=[bbk], writes=[by])
                dv = yb[g * 256:(g + 1) * 256, t0:t0 + TT].rearrange("(j p) t -> p j t", p=128)
                c.dma(dv, y[:], reads=[by], sem_buf=by)


def attn_phase(c, K, qk, v, kbar, attnT, T):
    nc = c.nc
    NQ = T // 128
    scale = 128.0 ** -0.5
    vv = v.rearrange("(c p) f -> p c f", p=128)
    with c.phase():
        qr = Ring(c, "at_q", [128, T], BF16, 2)
        kr = Ring(c, "at_k", [128, T], BF16, 2)
        vr = Ring(c, "at_v", [128, NQ, 128], BF16, 2)
        kbr = Ring(c, "at_kb", [128, 16], BF16, 2)
        gm = c.sbuf("at_gm", [128, NQ, 16], F32)
        bgm = Buf("at_gm")
        top = c.sbuf("at_top", [128, NQ, 8], F32)
        btop = Buf("at_top")
        thr = c.sbuf("at_thr", [128, NQ], F32)
        bthr = Buf("at_thr")
        pen = c.sbuf("at_pen", [128, NQ, 16], BF16)
        bpen = Buf("at_pen")
        penT = c.sbuf("at_penT", [128, T], BF16)
        bpenT = Buf("at_penT")
        c.op("pool", lambda: nc.gpsimd.memset(penT[:], 0.0), writes=[bpenT])
        ptr = Ring(c, "at_pt", [128, 4, 128], BF16, 3)
        rir = Ring(c, "at_ri", [128, 128], F32, 2)
        osr = Ring(c, "at_os", [128, T], BF16, 2)
        gbank = c.psum("at_gb", [128, 512], F32)
        bgbank = Buf("at_gb")
        tbank = c.psum("at_tb", [16, 1024], BF16)
        btbank = Buf("at_tb")
        sbk = Ring(c, "at_s", [128, 4, 128], F32, 2, psum=True)
        obk = Ring(c, "at_o", [128, 512], F32, 2, psum=True)
        rbk = Ring(c, "at_r", [128, 512], F32, 2, psum=True)

        heads = {}

        def load_head(h):
            q, bq = qr.next()
            k, bk_ = kr.next()
            vt, bv = vr.next()
            kb, bkb = kbr.next()
            c.dma(q[:], qk[h * 128:(h + 1) * 128, :], writes=[bq], sem_buf=bq)
            c.dma(k[:], qk[1024 + h * 128:1024 + (h + 1) * 128, :], writes=[bk_], sem_buf=bk_)
            for c0 in range(0, NQ, 8):
                c.dma(vt[:, c0:c0 + 8, :], vv[:, c0:c0 + 8, h * 128:(h + 1) * 128], writes=[bv], sem_buf=bv)
            c.dma(kb[:], kbar[:, h * 16:(h + 1) * 16], writes=[bkb], sem_buf=bkb)
            heads[h] = (q, bq, k, bk_, vt, bv, kb, bkb)

        load_head(0)
        for h in range(NH):
            if h + 1 < NH:
                load_head(h + 1)
            q, bq, k, bk_, vt, bv, kb, bkb = heads.pop(h)
            for i in range(NQ):
                c.op("pe", lambda: nc.tensor.matmul(gbank[:, i * 16:(i + 1) * 16], q[:, i * 128:(i + 1) * 128], kb[:],
                                                    start=True, stop=True), reads=[bq, bkb], writes=[bgbank])
            c.op("dve", lambda: nc.vector.tensor_tensor(gm[:].rearrange("p a b -> p (a b)"), gbank[:, 0:NQ * 16], K.negmask[:], ALU.add),
                 reads=[bgbank], writes=[bgm])
            for i in range(NQ):
                c.op("dve", lambda: nc.vector.max(out=top[:, i, :], in_=gm[:, i, :]), reads=[bgm], writes=[btop])
            c.op("dve", lambda: nc.vector.tensor_scalar(thr[:], top[:, :, 2], -1.0e29, None, ALU.max), reads=[btop], writes=[bthr])
            for i in range(NQ):
                c.op("dve", lambda: nc.vector.tensor_scalar(pen[:, i, :], gm[:, i, :], thr[:, i:i + 1], PEN, ALU.is_lt, ALU.mult),
                     reads=[bgm, bthr], writes=[bpen])
            for i0 in range(0, NQ, 8):
                for i in range(i0, min(i0 + 8, NQ)):
                    c.op("pe", lambda: nc.tensor.transpose(tbank[:, (i - i0) * 128:(i - i0 + 1) * 128], pen[:, i, :], K.ident[:]),
                         reads=[bpen], writes=[btbank])
                nn = min(8, NQ - i0) * 128
                c.op("act", lambda: nc.scalar.copy(penT[0:16, i0 * 128:i0 * 128 + nn], tbank[:, 0:nn]), reads=[btbank], writes=[bpenT])
            os_, bos = osr.next()
            G = []
            for i in range(NQ):
                chunks = list(range(i + 1))
                for c0 in range(0, len(chunks), 4):
                    G.append({"i": i, "grp": chunks[c0:c0 + 4], "first": c0 == 0, "last": c0 + 4 >= len(chunks)})
            acc = {}

            def emit_qk(g):
                i = g["i"]
                own = i // 2
                sb_, bsb = sbk.next()
                g["s"] = (sb_, bsb)
                qs = q[:, i * 128:(i + 1) * 128]
                for jj, j in enumerate(g["grp"]):
                    past = (j // 2) < own
                    c.op("pe", lambda: nc.tensor.matmul(sb_[:, jj, :], k[:, j * 128:(j + 1) * 128], qs, start=True, stop=not past),
                         reads=[bk_, bq], writes=[bsb])
                    if past:
                        n = j // 2
                        c.op("pe", lambda: nc.tensor.matmul(sb_[:, jj, :], K.E[:, n * 128:(n + 1) * 128], penT[:, i * 128:(i + 1) * 128],
                                                            start=False, stop=True), reads=[bpenT], writes=[bsb])

            def emit_exp(g):
                i = g["i"]
                sb_, bsb = g["s"]
                pt, bpt = ptr.next()
                g["p"] = (pt, bpt)
                for jj, j in enumerate(g["grp"]):
                    col = h * NQ + (i - j)
                    c.op("act", lambda: nc.scalar.activation(pt[:, jj, :], sb_[:, jj, :], AF.Exp, bias=K.alibi[:, col:col + 1], scale=scale),
                         reads=[bsb], writes=[bpt])
                    if j == i:
                        c.op("pool", lambda: nc.gpsimd.tensor_tensor(pt[:, jj, :], pt[:, jj, :], K.tri[:], ALU.mult),
                             reads=[bpt], writes=[bpt])

            def emit_pv(g):
                i = g["i"]
                pt, bpt = g["p"]
                if g["first"]:
                    acc[i] = (obk.next(), rbk.next())
                (ob, bob), (rb, brb) = acc[i]
                for jj, j in enumerate(g["grp"]):
                    st = g["first"] and jj == 0
                    last = (j == i)
                    c.op("pe", lambda: nc.tensor.matmul(ob[:, 0:128], vt[:, j, :], pt[:, jj, :], start=st, stop=last),
                         reads=[bv, bpt], writes=[bob])
                    c.op("pe", lambda: nc.tensor.matmul(rb[:, 0:128], K.ones_b[:], pt[:, jj, :], start=st, stop=last),
                         reads=[bpt], writes=[brb])
                if g["last"]:
                    ri, bri = rir.next()
                    c.op("dve", lambda: nc.vector.reciprocal(ri[:], rb[:, 0:128]), reads=[brb], writes=[bri])
                    c.op("dve", lambda: nc.vector.tensor_tensor(os_[:, i * 128:(i + 1) * 128], ob[:, 0:128], ri[:], ALU.mult),
                         reads=[bob, bri], writes=[bos])
                    del acc[i]

            emit_qk(G[0])
            for n in range(len(G)):
                if n + 1 < len(G):
                    emit_qk(G[n + 1])
                emit_exp(G[n])
                emit_pv(G[n])
            c.dma(attnT[h * 128:(h + 1) * 128, :], os_[:], reads=[bos], sem_buf=bos)


def merge_phase(c, ya, yb, yc, wA, wB, wC, sg, mT, T):
    nc = c.nc
    TS = 1024
    srcs = [y.rearrange("(k p) t -> p k t", p=128) for y in (ya, yb, yc)]
    ws = (wA, wB, wC)
    sgv = sg.rearrange("(b d) t -> d b t", b=3)
    with c.phase():
        act = Ring(c, "mg_a", [128, 3, 8, TS], BF16, 1)
        wt = Ring(c, "mg_w", [128, 3, 8, 256], BF16, 3)
        gr = Ring(c, "mg_g", [128, 3, 512], BF16, 3)
        tr = Ring(c, "mg_t", [128, 3, 512], F32, 2)
        mr = Ring(c, "mg_m", [128, 512], BF16, 3)
        banks = Ring(c, "mg_bk", [128, 512], F32, 6, psum=True)
        nsup = T // TS
        seq = [(su, s) for su in range(nsup) for s in range(8)]
        acts, wts = {}, {}

        def load_act(su):
            a, ba = act.next()
            for b in range(3):
                c.dma(a[:, b, :, :], srcs[b][:, :, su * TS:(su + 1) * TS], writes=[ba], sem_buf=ba)
            acts[su] = (a, ba)

        def load_w(i):
            w, bw = wt.next()
            for b in range(3):
                c.dma(w[:, b, :, :], ws[b][seq[i][1]], writes=[bw], sem_buf=bw)
            wts[i] = (w, bw)

        units = [(su, s, sb, j) for (su, s) in seq for sb in range(TS // 512) for j in range(2)]
        gts = {}

        def load_g(n):
            su_, s_, sb_, j_ = units[n]
            g, bg_ = gr.next()
            d0_ = s_ * 256 + j_ * 128
            t0_ = su_ * TS + sb_ * 512
            c.dma(g[:], sgv[d0_:d0_ + 128, :, t0_:t0_ + 512], writes=[bg_], sem_buf=bg_)
            gts[n] = (g, bg_)

        un = 0
        load_g(0)
        load_g(1)
        load_w(0)
        load_w(1)
        for i, (su, s) in enumerate(seq):
            if su not in acts:
                load_act(su)
            if i + 2 < len(seq):
                load_w(i + 2)
            w, bw = wts.pop(i)
            a, ba = acts[su]
            if c.bg is not None:
                c.bg.step(c.bg_rate)
            for sb in range(TS // 512):
                t0 = su * TS + sb * 512
                for j in range(2):
                    d0 = s * 256 + j * 128
                    assert units[un] == (su, s, sb, j)
                    if un + 2 < len(units):
                        load_g(un + 2)
                    g, bg = gts.pop(un)
                    un += 1
                    bl = [banks.next() for _ in range(3)]
                    for b in range(3):
                        for k in range(8):
                            c.op("pe", lambda: nc.tensor.matmul(bl[b][0][:], w[:, b, k, j * 128:(j + 1) * 128],
                                                                a[:, b, k, sb * 512:(sb + 1) * 512],
                                                                start=(k == 0), stop=(k == 7)), reads=[ba, bw], writes=[bl[b][1]])
                    t, bt = tr.next()
                    m, bm = mr.next()
                    for b in range(3):
                        c.op("dve", lambda: nc.vector.tensor_tensor(t[:, b, :], bl[b][0][:], g[:, b, :], ALU.mult),
                             reads=[bl[b][1], bg], writes=[bt])
                    c.op("dve", lambda: nc.vector.tensor_tensor(t[:, 0, :], t[:, 0, :], t[:, 1, :], ALU.add), reads=[bt], writes=[bt])
                    c.op("dve", lambda: nc.vector.tensor_tensor(m[:], t[:, 0, :], t[:, 2, :], ALU.add), reads=[bt], writes=[bm])
                    c.dma(mT[d0:d0 + 128, t0:t0 + 512], m[:], reads=[bm], sem_buf=bm)


def setup_consts(c, nc, K, T, din):
    NQ = T // 128
    gains_d = din("gains", [128, (3 * NL + 1) * 16])
    convw_d = din("convw", [128, NL * 8 * 3])
    pscale_d = din("pscale", [128, NL * 8])
    invd_d = din("invd", [128, 4 * 512])
    negmask_d = din("negmask", [128, NQ * 16])
    alibi_d = din("alibi", [128, NH * NQ])
    tri_d = din("tri", [128, 128])
    ident_d = din("ident", [128, 128])
    E_d = din("Emat", [128, 16 * 128])
    specs = [("gains", gains_d, [128, (3 * NL + 1) * 16], F32), ("convw", convw_d, [128, NL * 8 * 3], F32),
             ("pscale", pscale_d, [128, NL * 8], F32),
             ("invd", invd_d.rearrange("p (g t) -> p g t", g=4), [128, 4, 512], F32),
             ("negmask", negmask_d, [128, NQ * 16], F32), ("alibi", alibi_d, [128, NH * NQ], F32),
             ("tri", tri_d, [128, 128], BF16), ("ident", ident_d, [128, 128], BF16),
             ("E", E_d, [128, 16 * 128], BF16)]
    for (name, src, shape, dt) in specs:
        setattr(K, name, c.sbuf("k_" + name, shape, dt, persistent=True))
    K.ones_f = c.sbuf("k_ones_f", [128, 128], F32, persistent=True)
    K.ones_b = c.sbuf("k_ones_b", [128, 128], BF16, persistent=True)
    with c.phase():
        for (name, src, shape, dt) in specs:
            t = getattr(K, name)
            b = Buf("k_" + name)
            if dt == F32:
                c.dma(t[:], src, writes=[b], sem_buf=b)
            else:
                st = c.sbuf("ks_" + name, shape, F32)
                bs = Buf("ks_" + name)
                c.dma(st[:], src, writes=[bs], sem_buf=bs)
                c.op("dve", lambda: nc.vector.tensor_copy(t[:], st[:]), reads=[bs], writes=[b])
        c.op("dve", lambda: nc.vector.memset(K.ones_f[:], 1.0))
        c.op("dve", lambda: nc.vector.memset(K.ones_b[:], 1.0))


class Consts:
    pass


def build(T, stop_after=None, dbg=(), skip_ffn1=False):
    NQ = T // 128
    nc = bass.Bass("TRN2", target_bir_lowering=False)

    def din(name, shape):
        return nc.dram_tensor(name, list(shape), F32, kind="ExternalInput").ap()

    def scr(name, shape, dt):
        kind = "ExternalOutput" if name in dbg else "Internal"
        return nc.dram_tensor(name, list(shape), dt, kind=kind).ap()

    xT = din("xT", [D, T])
    W = {}
    W["ffn1_w_in"] = din("ffn1_w_in", [NL, D, 2 * DFF])
    W["ffn1_w_out"] = din("ffn1_w_out", [NL, DFF, D])
    W["mix_w_in"] = din("mix_w_in", [NL, D, INDIM])
    W["conv_w_out"] = din("conv_w_out", [NL, 1024, D])
    W["pool_w"] = din("pool_w", [NL, 4, 256, 256])
    W["pool_w_out"] = din("pool_w_out", [NL, 1024, D])
    W["attn_w_out"] = din("attn_w_out", [NL, 1024, D])
    W["mix_w_out"] = din("mix_w_out", [NL, D, D])
    W["ffn2_w_in"] = din("ffn2_w_in", [NL, D, 2 * DFF])
    W["ffn2_w_out"] = din("ffn2_w_out", [NL, DFF, D])
    yT = nc.dram_tensor("yT", [D, T], F32, kind="ExternalOutput").ap()

    S = {}
    for l in range(NL):
        S["f1i", l] = scr("s_f1i%d" % l, [22, 128, 16, 512], BF16)
        S["f1o", l] = scr("s_f1o%d" % l, [8, 128, 44, 256], BF16)
        S["mi", l] = scr("s_mi%d" % l, [26, 128, 16, 512], BF16)
        S["cwo", l] = scr("s_cwo%d" % l, [8, 128, 8, 256], BF16)
        S["pwo", l] = scr("s_pwo%d" % l, [8, 128, 8, 256], BF16)
        S["awo", l] = scr("s_awo%d" % l, [8, 128, 8, 256], BF16)
        S["mo", l] = scr("s_mo%d" % l, [8, 128, 16, 256], BF16)
        S["f2i", l] = scr("s_f2i%d" % l, [22, 128, 16, 512], BF16)
        S["f2o", l] = scr("s_f2o%d" % l, [8, 128, 44, 256], BF16)
    xres = scr("xres", [D, T], F32)
    hT = scr("hT", [D, T], BF16)
    gT = scr("gT", [DFF, T], BF16)
    pa = scr("pa", [4096, T], F32)
    qk = scr("qk", [2048, T], BF16)
    vS = scr("vS", [T, 1024], BF16)
    kbar = scr("kbar", [128, NH * 16], BF16)
    sgS = scr("sgS", [3 * D, T], BF16)
    ya = scr("ya", [1024, T], BF16)
    yb = scr("yb", [1024, T], BF16)
    yc = scr("yc", [1024, T], BF16)
    mT = scr("mT", [D, T], BF16)

    c = Ctx(nc)
    K = Consts()
    bg = BgCast(c)

    class Stop(Exception):
        pass

    def check(name):
        if stop_after == name:
            raise Stop()

    ffn_in_runs = [(0, 22, 256, 0, 0), (DFF, 22, 256, 0, 256)]
    std8 = [(0, 8, 256, 0, 0)]
    for l in range(NL):
        if not skip_ffn1:
            bg.add(("f1i", l), W["ffn1_w_in"][l], D, ffn_in_runs, S["f1i", l])
            bg.add(("f1o", l), W["ffn1_w_out"][l], DFF, std8, S["f1o", l])
        bg.add(("mi", l), W["mix_w_in"][l], D, [(0, 26, 512, 0, 0)], S["mi", l])
        bg.add(("cwo", l), W["conv_w_out"][l], 1024, std8, S["cwo", l])
        bg.add(("pwo", l), W["pool_w_out"][l], 1024, std8, S["pwo", l])
        bg.add(("awo", l), W["attn_w_out"][l], 1024, std8, S["awo", l])
        bg.add(("mo", l), W["mix_w_out"][l], D, std8, S["mo", l])
        bg.add(("f2i", l), W["ffn2_w_in"][l], D, ffn_in_runs, S["f2i", l])
        bg.add(("f2o", l), W["ffn2_w_out"][l], DFF, std8, S["f2o", l])
    c.bg = bg
    c.bg_rate = 2

    def need(*keys):
        todo = [k for k in keys if bg.left.get(k, 0) > 0]
        if todo:
            with c.phase():
                for k in todo:
                    bg.ensure(k)
                while bg.loaded or bg.casted:
                    if bg.casted:
                        bg._store()
                    if bg.loaded:
                        bg._cast()

    try:
        setup_consts(c, nc, K, T, din)
        check("consts")
        xcur = xT
        for l in range(NL):
            if not skip_ffn1:
                need(("f1i", l))
                norm_phase(c, K, xcur, hT, BF16, K.gains[:, (l * 3 + 0) * 16:(l * 3 + 1) * 16], T)
                check("norm1")
                linear_phase(c, hT, D, S["f1i", l], 22, 512, 1024, T, EpiFfnIn(gT))
                check("ffn1_in")
                need(("f1o", l))
                linear_phase(c, gT, DFF, S["f1o", l], 8, 256, 1024, T, EpiResid(xcur, xres, 0.5), nact=1)
                xcur = xres
                check("ffn1")
            norm_phase(c, K, xcur, hT, BF16, K.gains[:, (l * 3 + 1) * 16:(l * 3 + 2) * 16], T)
            need(("mi", l))
            linear_phase(c, hT, D, S["mi", l], 26, 512, 1024, T, EpiMixIn(pa, qk, vS, kbar, sgS, T), tokmajor=(12, 13))
            check("mix_in")
            conv_phase(c, K, l, pa, ya, T)
            check("conv")
            pool_phase(c, K, l, pa, W["pool_w"][l], yb, T)
            check("pool")
            attn_phase(c, K, qk, vS, kbar, yc, T)
            check("attn")
            need(("cwo", l), ("pwo", l), ("awo", l))
            merge_phase(c, ya, yb, yc, S["cwo", l], S["pwo", l], S["awo", l], sgS, mT, T)
            check("merge")
            need(("mo", l))
            linear_phase(c, mT, D, S["mo", l], 8, 256, 1024, T, EpiResid(xcur, xres, 1.0))
            xcur = xres
            check("mix")
            norm_phase(c, K, xres, hT, BF16, K.gains[:, (l * 3 + 2) * 16:(l * 3 + 3) * 16], T)
            need(("f2i", l))
            linear_phase(c, hT, D, S["f2i", l], 22, 512, 1024, T, EpiFfnIn(gT))
            need(("f2o", l))
            linear_phase(c, gT, DFF, S["f2o", l], 8, 256, 1024, T, EpiResid(xres, xres, 0.5), nact=1)
            check("layer%d" % l)
        norm_phase(c, K, xres, yT, F32, K.gains[:, 3 * NL * 16:(3 * NL + 1) * 16], T)
    except Stop:
        pass
    c.barrier()
    c.es.close()
    return nc, c


def host_consts(T):
    NQ = T // 128
    k = {}
    t = np.arange(512, dtype=np.float32)
    invd = np.stack([1.0 / np.minimum(t + 1.0, float(w)) for w in (2, 4, 8, 16)], 0)
    k["invd"] = np.ascontiguousarray(np.broadcast_to(invd.reshape(1, 4 * 512), (128, 4 * 512))).astype(np.float32)
    nm = np.zeros((NQ, 16), np.float32)
    for i in range(NQ):
        nm[i, (i // 2):] = NEG
    k["negmask"] = np.ascontiguousarray(np.broadcast_to(nm.reshape(1, NQ * 16), (128, NQ * 16))).astype(np.float32)
    slopes = np.exp2(-(8.0 / NH) * np.arange(1, NH + 1, dtype=np.float64))
    ki = np.arange(128, dtype=np.float64)[:, None, None]
    rel = np.arange(NQ, dtype=np.float64)[None, None, :]
    al = slopes[None, :, None] * (ki - rel * 128.0 - 64.0)
    k["alibi"] = np.ascontiguousarray(al.reshape(128, NH * NQ)).astype(np.float32)
    kk = np.arange(128)
    k["tri"] = (kk[:, None] <= kk[None, :]).astype(np.float32)
    k["ident"] = np.eye(128, dtype=np.float32)
    E = np.zeros((128, 16, 128), np.float32)
    for n in range(16):
        E[n, n, :] = 1.0
    k["Emat"] = np.ascontiguousarray(E.reshape(128, 16 * 128))
    return k


def chunked(vec):
    return np.ascontiguousarray(np.asarray(vec, np.float32).reshape(-1, 128).T)


def host_small(inp):
    g = []
    for l in range(NL):
        g += [chunked(inp["ffn1_norm"][l]), chunked(inp["mix_norm"][l]), chunked(inp["ffn2_norm"][l])]
    g.append(chunked(inp["final_norm"]))
    out = {"gains": np.ascontiguousarray(np.concatenate(g, axis=1))}
    cw = np.asarray(inp["conv_w"], np.float32)
    cw = cw.reshape(NL, 3, 8, 128).transpose(3, 0, 2, 1)
    out["convw"] = np.ascontiguousarray(cw.reshape(128, NL * 8 * 3))
    ps = np.asarray(inp["pool_scale"], np.float32).reshape(NL, 8, 128).transpose(2, 0, 1)
    out["pscale"] = np.ascontiguousarray(ps.reshape(128, NL * 8))
    return out


WNAMES = ["ffn1_w_in", "ffn1_w_out", "mix_w_in", "conv_w_out", "pool_w", "pool_w_out", "attn_w_out",
          "mix_w_out", "ffn2_w_in", "ffn2_w_out"]


def kernel(**inputs):
    x = np.asarray(inputs["x"], np.float32)
    B, T, _ = x.shape
    nc, _ = build(T)
    base = {n: np.ascontiguousarray(np.asarray(inputs[n], np.float32)) for n in WNAMES}
    base.update(host_consts(T))
    base.update(host_small(inputs))
    in_maps = []
    for b in range(B):
        m = dict(base)
        m["xT"] = np.ascontiguousarray(x[b].T)
        in_maps.append(m)
    res = run_bass_kernel_spmd(nc, in_maps, core_ids=list(range(B)))
    out = np.stack([np.ascontiguousarray(res.results[b]["yT"].T) for b in range(B)], 0)
    return out.astype(np.float32)
```
